# Optimizing a Trainium2 kernel written in Bass

```python
import jax, jax.numpy as jnp
from jax import lax
import numpy as np

D_MODEL = 1024
BATCH = 2
SEQ = 16384
DEPTH = 2

N_HEADS = 8
N_KV_HEADS = 2
HEAD_DIM = 64
ATTN_WIDTH = N_HEADS * HEAD_DIM
KV_WIDTH = N_KV_HEADS * HEAD_DIM
WINDOW = 128
BLOCK = 128
N_BUCKETS = 32
MAX_DISTANCE = 128
N_FOURIER_GROUPS = 4
FOURIER_GROUP_DIM = 128
FOURIER_WIDTH = N_FOURIER_GROUPS * FOURIER_GROUP_DIM
N_BRANCHES = 2
IN_WIDTH = ATTN_WIDTH + 2 * KV_WIDTH + FOURIER_WIDTH + N_BRANCHES * D_MODEL
N_EXPERTS = 16
CAPACITY_FACTOR = 2
D_FF = 2816
EPS = 1e-6
NEG_INF = -1e30

kernel_name = "hybrid_swa_fnet_ec_moe_encoder"


def rms_norm(x, g):
    xf = x.astype(jnp.float32)
    y = xf * lax.rsqrt(jnp.mean(xf * xf, axis=-1, keepdims=True) + EPS)
    return (y * g.astype(jnp.float32)).astype(x.dtype)


def t5_bucket(rel):
    half = N_BUCKETS // 2
    max_exact = half // 2
    ret = (rel > 0).astype(jnp.int32) * half
    n = jnp.abs(rel)
    nf = jnp.maximum(n, 1).astype(jnp.float32)
    large = max_exact + (jnp.log(nf / max_exact) / np.float32(np.log(MAX_DISTANCE / max_exact))
                         * (half - max_exact)).astype(jnp.int32)
    large = jnp.minimum(large, half - 1)
    return ret + jnp.where(n < max_exact, n, large)


def window_attention(q, k, v, sink, rel_bias):
    b, s, _ = q.shape
    nb = s // BLOCK
    g = N_HEADS // N_KV_HEADS
    qb = q.reshape(b, nb, BLOCK, N_KV_HEADS, g, HEAD_DIM)

    def windows(t):
        t = t.reshape(b, s, N_KV_HEADS, HEAD_DIM)
        t = jnp.pad(t, ((0, 0), (BLOCK, BLOCK), (0, 0), (0, 0)))
        t = t.reshape(b, nb + 2, BLOCK, N_KV_HEADS, HEAD_DIM)
        return jnp.concatenate([t[:, :-2], t[:, 1:-1], t[:, 2:]], axis=2)

    kw, vw = windows(k), windows(v)
    q_loc = jnp.arange(BLOCK)
    k_loc = jnp.arange(3 * BLOCK) - BLOCK
    rel = k_loc[None, :] - q_loc[:, None]
    bias = rel_bias[t5_bucket(rel)].astype(jnp.float32)
    bias = bias.transpose(2, 0, 1).reshape(N_KV_HEADS, g, BLOCK, 3 * BLOCK)
    k_abs = jnp.arange(nb)[:, None] * BLOCK + k_loc[None, :]
    valid = (jnp.abs(rel) <= WINDOW)[None] & ((k_abs >= 0) & (k_abs < s))[:, None, :]

    scores = jnp.einsum('bnqkgd,bnskd->bnkgqs', qb, kw,
                        preferred_element_type=jnp.float32) * (HEAD_DIM ** -0.5) + bias
    scores = jnp.where(valid[None, :, None, None], scores, NEG_INF)
    sink_l = sink.astype(jnp.float32).reshape(N_KV_HEADS, g)[:, :, None]
    m = jnp.maximum(scores.max(axis=-1), sink_l)
    p = jnp.exp(scores - m[..., None])
    denom = p.sum(axis=-1, keepdims=True) + jnp.exp(sink_l - m)[..., None]
    probs = (p / denom).astype(v.dtype)
    out = jnp.einsum('bnkgqs,bnskd->bnqkgd', probs, vw)
    return out.reshape(b, s, ATTN_WIDTH)


def fourier_mix(f):
    b, s, _ = f.shape
    fg = f.reshape(b, s, N_FOURIER_GROUPS, FOURIER_GROUP_DIM).astype(jnp.float32)
    y = jnp.fft.fft2(fg, axes=(1, 3), norm="ortho").real
    return y.reshape(b, s, FOURIER_WIDTH).astype(f.dtype)


def expert_choice_ffn(h, w_router, w_gate, w_up, w_down):
    b, s, d = h.shape
    cap = CAPACITY_FACTOR * s // N_EXPERTS
    logits = jnp.einsum('bsd,de->bse', h, w_router, preferred_element_type=jnp.float32)
    affinity = jax.nn.softmax(logits, axis=-1)
    gates, idx = lax.top_k(affinity.transpose(0, 2, 1), cap)
    xs = jax.vmap(lambda t, i: t[i])(h, idx)
    hg = jnp.einsum('becd,edf->becf', xs, w_gate)
    hu = jnp.einsum('becd,edf->becf', xs, w_up)
    y = jnp.einsum('becf,efd->becd', jax.nn.silu(hg) * hu, w_down) * gates[..., None].astype(h.dtype)
    return jax.vmap(lambda i, val: jnp.zeros((s, d), h.dtype).at[i.reshape(-1)].add(val.reshape(-1, d)))(idx, y)


def setup_inputs(seed: int = 0) -> dict:
    key = jax.random.key(seed)
    ks = jax.random.split(key, 14)
    f32 = jnp.float32
    nrm = lambda k, shape, fan_in: jax.random.normal(k, shape, f32) * (fan_in ** -0.5)
    return {
        "x": jax.random.normal(ks[0], (BATCH, SEQ, D_MODEL), f32),
        "rel_bias": jax.random.normal(ks[1], (N_BUCKETS, N_HEADS), f32) * 0.1,
        "g_mix": 1.0 + 0.02 * jax.random.normal(ks[2], (DEPTH, D_MODEL), f32),
        "w_in": nrm(ks[3], (DEPTH, D_MODEL, IN_WIDTH), D_MODEL),
        "attn_sink": jax.random.normal(ks[4], (DEPTH, N_HEADS), f32) * 0.5,
        "w_attn_proj": nrm(ks[5], (DEPTH, ATTN_WIDTH, D_MODEL), ATTN_WIDTH),
        "w_fourier_proj": nrm(ks[6], (DEPTH, FOURIER_WIDTH, D_MODEL), FOURIER_WIDTH),
        "w_out": nrm(ks[7], (DEPTH, D_MODEL, D_MODEL), D_MODEL),
        "g_ffn": 1.0 + 0.02 * jax.random.normal(ks[8], (DEPTH, D_MODEL), f32),
        "w_router": nrm(ks[9], (DEPTH, D_MODEL, N_EXPERTS), D_MODEL),
        "w_exp_gate": nrm(ks[10], (DEPTH, N_EXPERTS, D_MODEL, D_FF), D_MODEL),
        "w_exp_up": nrm(ks[11], (DEPTH, N_EXPERTS, D_MODEL, D_FF), D_MODEL),
        "w_exp_down": nrm(ks[12], (DEPTH, N_EXPERTS, D_FF, D_MODEL), D_FF),
        "g_final": 1.0 + 0.02 * jax.random.normal(ks[13], (D_MODEL,), f32),
    }


def reference(x, rel_bias, g_mix, w_in, attn_sink, w_attn_proj, w_fourier_proj, w_out,
              g_ffn, w_router, w_exp_gate, w_exp_up, w_exp_down, g_final):
    splits = np.cumsum([ATTN_WIDTH, KV_WIDTH, KV_WIDTH, FOURIER_WIDTH, D_MODEL]).tolist()
    for l in range(DEPTH):
        h = rms_norm(x, g_mix[l])
        z = jnp.einsum('bsd,de->bse', h, w_in[l])
        q, k, v, f, gate_a, gate_b = jnp.split(z, splits, axis=-1)
        a = jnp.einsum('bse,ed->bsd', window_attention(q, k, v, attn_sink[l], rel_bias), w_attn_proj[l])
        fo = jnp.einsum('bse,ed->bsd', fourier_mix(f), w_fourier_proj[l])
        merged = jax.nn.sigmoid(gate_a) * a + jax.nn.sigmoid(gate_b) * fo
        x = x + jnp.einsum('bsd,de->bse', merged, w_out[l])
        h2 = rms_norm(x, g_ffn[l])
        x = x + expert_choice_ffn(h2, w_router[l], w_exp_gate[l], w_exp_up[l], w_exp_down[l])
    return rms_norm(x, g_final)
```

```python
import numpy as np
import ml_dtypes
from contextlib import ExitStack
import concourse.bass as bass
import concourse.mybir as mybir
from concourse.bass_utils import run_bass_kernel_spmd

F32 = mybir.dt.float32
BF16 = mybir.dt.bfloat16
I32 = mybir.dt.int32
U32 = mybir.dt.uint32
ALU = mybir.AluOpType
AF = mybir.ActivationFunctionType
AX = mybir.AxisListType
NPBF = ml_dtypes.bfloat16

D = 1024
SEQ = 16384
NB = 2
DEPTH = 2
TOK = 4096
NCORES = 8
INW = 3328
DFF = 2816
NEXP = 16
CAP = 2048
EPS = 1e-6
NEG = -30000.0
NDMA = 40


class Buf:
    __slots__ = ("t", "w", "r", "name", "ws")

    def __init__(self, t, name=""):
        self.t = t
        self.w = None
        self.r = {}
        self.name = name
        self.ws = []

    def __getitem__(self, idx):
        return self.t[idx]


class Sched:
    def __init__(self, nc, stack):
        self.nc = nc
        self.stack = stack
        self.eng = {"pe": nc.tensor, "act": nc.scalar, "dve": nc.vector,
                    "pool": nc.gpsimd, "sp": nc.sync}
        self.sem = {k: stack.enter_context(nc.semaphore("s_" + k)) for k in self.eng}
        self.cnt = {k: 0 for k in self.eng}
        self.seen = {k: {} for k in self.eng}
        self.dsem = [stack.enter_context(nc.semaphore("d%d" % i)) for i in range(NDMA)]
        self.dval = [0] * NDMA
        self.drr = 0
        self.ninst = 0
        self.prefix = ""

    def sb(self, shape, dt, name):
        t = self.stack.enter_context(self.nc.sbuf_tensor(self.prefix + name, list(shape), dt))
        return Buf(t, name)

    def ps(self, shape, dt, name):
        t = self.stack.enter_context(self.nc.psum_tensor(self.prefix + name, list(shape), dt))
        return Buf(t, name)

    def barrier(self):
        snap = dict(self.cnt)
        for e in self.eng:
            for e2 in self.eng:
                if e2 != e:
                    self._wait(e, e2, snap[e2])
            for i in range(NDMA):
                self._wait(e, ("dma", i), self.dval[i])

    def _semobj(self, key):
        if isinstance(key, str):
            return self.sem[key]
        return self.dsem[key[1]]

    def _wait(self, eng, key, val):
        if val <= 0:
            return
        if self.seen[eng].get(key, 0) >= val:
            return
        self.eng[eng].wait_ge(self._semobj(key), val)
        self.seen[eng][key] = val

    def _deps(self, eng, reads, writes):
        deps = {}

        def add(t):
            if t is None:
                return
            k, v = t
            if deps.get(k, 0) < v:
                deps[k] = v
        for b in reads:
            add(b.w)
            for t in b.ws:
                add(t)
        for b in writes:
            add(b.w)
            for t in b.ws:
                add(t)
            for k, v in b.r.items():
                add((k, v))
        for k, v in deps.items():
            if k == eng and eng == "pe":
                continue
            self._wait(eng, k, v)

    def _mark(self, t, reads, writes):
        k, v = t
        for b in reads:
            if b.r.get(k, 0) < v:
                b.r[k] = v
        for b in writes:
            b.w = t
            b.r = {}
            b.ws = []

    def op(self, eng, fn, reads=(), writes=(), inc=True):
        self._deps(eng, reads, writes)
        inst = fn(self.eng[eng])
        if inc:
            self.cnt[eng] += 1
            inst.then_inc(self.sem[eng], 1)
            self._mark((eng, self.cnt[eng]), reads, writes)
        else:
            self._mark((eng, self.cnt[eng] + 1), reads, writes)
        self.ninst += 1
        return inst

    def dma(self, q, fn, reads=(), writes=(), also_writes=()):
        i = self.drr
        self.drr = (self.drr + 1) % NDMA
        key = ("dma", i)
        self._wait(q, key, self.dval[i])
        self._deps(q, reads, writes)
        inst = fn(self.eng[q])
        self.dval[i] += 16
        inst.then_inc(self.dsem[i], 16)
        self._mark((key, self.dval[i]), reads, writes)
        for b in also_writes:
            b.ws.append((key, self.dval[i]))
        self.ninst += 1
        return inst

    def finish(self, bufs, eng="sp"):
        for b in bufs:
            if b.w is not None:
                self._wait(eng, b.w[0], b.w[1])

    def finish_all(self, eng="sp"):
        for i in range(NDMA):
            self._wait(eng, ("dma", i), self.dval[i])


class Rot:
    def __init__(self, S, n, shape, dt, name, psum=False):
        mk = S.ps if psum else S.sb
        self.bufs = [mk(shape, dt, "%s%d" % (name, i)) for i in range(n)]
        self.i = 0

    def next(self):
        b = self.bufs[self.i]
        self.i = (self.i + 1) % len(self.bufs)
        return b


class Ctx:
    nc = None
    S = None
    nphase = 0


def _D(nc, io, name, shape, dt, kind):
    if io is not None:
        return io[name]
    return nc.dram_tensor(name, list(shape), dt, kind=kind).ap()


def _S(ctx, nc, st):
    if ctx is None:
        return Sched(nc, st)
    ctx.S.stack = st
    ctx.nphase += 1
    ctx.S.prefix = "p%d_" % ctx.nphase
    return ctx.S


def _end(ctx, S):
    if ctx is None:
        S.finish_all("sp")
    else:
        S.barrier()


def emit_norm_T(S, x_rows_ap, xblk, xn, tp_ps, hT, hT_dep, col0, g_sb, gcols, ssq, rstd, junk):
    S.dma("sp", lambda e: e.dma_start(out=xblk[:], in_=x_rows_ap), writes=[xblk])
    S.op("act", lambda e: e.activation(out=junk[:], in_=xblk[:], func=AF.Square,
                                       accum_out=ssq[:, 0:1]), reads=[xblk], writes=[junk, ssq])
    S.op("dve", lambda e: e.tensor_scalar(out=rstd[:, 0:1], in0=ssq[:, 0:1], scalar1=1.0 / D,
                                          scalar2=EPS, op0=ALU.mult, op1=ALU.add),
         reads=[ssq], writes=[rstd])
    S.op("act", lambda e: e.activation(out=rstd[:, 0:1], in_=rstd[:, 0:1], func=AF.Sqrt),
         reads=[rstd], writes=[rstd])
    S.op("dve", lambda e: e.reciprocal(out=rstd[:, 0:1], in_=rstd[:, 0:1]), reads=[rstd], writes=[rstd])
    S.op("dve", lambda e: e.tensor_scalar(out=xn[:], in0=xblk[:], scalar1=rstd[:, 0:1],
                                          scalar2=None, op0=ALU.mult), reads=[xblk, rstd], writes=[xn])
    for c in range(8):
        S.op("pe", lambda e, c=c: e.transpose(out=tp_ps[:, c, :], in_=xn[:, c * 128:(c + 1) * 128],
                                              identity=gcols["ident"][:]),
             reads=[xn, gcols["ident"]], writes=[tp_ps])
    S.op("dve", lambda e: e.tensor_tensor(out=hT[:, :, col0:col0 + 128], in0=tp_ps[:],
                                          in1=g_sb[:].unsqueeze(2).to_broadcast([128, 8, 128]),
                                          op=ALU.mult),
         reads=[tp_ps, g_sb], writes=[hT_dep])


def build_A(ctx=None, io=None):
    nc = ctx.nc if ctx is not None else bass.Bass("TRN2", target_bir_lowering=False)
    xe = _D(nc, io, "xe", [TOK + 256, D], F32, "ExternalInput")
    w_in = _D(nc, io, "w_in", [D, INW], F32, "ExternalInput")
    g_in = _D(nc, io, "g", [128, 8], F32, "ExternalInput")
    biasg = _D(nc, io, "biasg", [128, 8, 3, 128], F32, "ExternalInput")
    maskw = _D(nc, io, "maskw", [128, 3, 128], F32, "ExternalInput")
    edges = _D(nc, io, "edges", [128, 2, 128], F32, "ExternalInput")
    sinkb = _D(nc, io, "sinkb", [128, 8], F32, "ExternalInput")
    w_ap = _D(nc, io, "w_ap", [512, D], F32, "ExternalInput")
    ident_in = _D(nc, io, "ident", [128, 128], F32, "ExternalInput")
    fT = _D(nc, io, "fT", [4, 128, TOK], BF16, "ExternalOutput")
    maT = _D(nc, io, "maT", [8, 128, TOK], F32, "ExternalOutput")
    sgbT = _D(nc, io, "sgbT", [8, 128, TOK], F32, "ExternalOutput")
    outd = Buf(None, "outs")

    with ExitStack() as st:
        S = _S(ctx, nc, st)
        wkv = S.sb([128, 8, 256], BF16, "wkv")
        wr = S.sb([128, 8, 3072], BF16, "wr")
        wap = S.sb([128, 4, D], BF16, "wap")
        g_sb = S.sb([128, 8], F32, "g_sb")
        ident = S.sb([128, 128], BF16, "identb")
        kT = S.sb([128, TOK + 256], BF16, "kT")
        vaug = S.sb([128, 34, 2, 65], BF16, "vaug")
        bm = S.sb([128, 8, 3, 128], BF16, "bm")
        bfst = S.sb([128, 8, 128], BF16, "bfst")
        blst = S.sb([128, 8, 128], BF16, "blst")
        esink = S.sb([128, 8], F32, "esink")
        tmpb = S.sb([128, 8, 3, 128], F32, "tmpb")
        tmpm = S.sb([128, 3, 128], F32, "tmpm")
        tmpe = S.sb([128, 2, 128], F32, "tmpe")
        gc = {"ident": ident}

        S.dma("pool", lambda e: e.dma_start(out=ident[:], in_=ident_in), writes=[ident])
        S.dma("sp", lambda e: e.dma_start(out=g_sb[:], in_=g_in), writes=[g_sb])
        S.dma("sp", lambda e: e.dma_start(out=esink[:], in_=sinkb), writes=[esink])
        S.dma("sp", lambda e: e.dma_start(out=tmpb[:], in_=biasg), writes=[tmpb])
        S.dma("sp", lambda e: e.dma_start(out=tmpm[:], in_=maskw), writes=[tmpm])
        S.dma("sp", lambda e: e.dma_start(out=tmpe[:], in_=edges), writes=[tmpe])
        w_in_v = w_in.rearrange("(c p) e -> p c e", p=128)
        S.dma("pool", lambda e: e.dma_start(out=wkv[:], in_=w_in_v[:, :, 512:768]), writes=[wkv])
        for j in range(4):
            S.dma("pool", lambda e, j=j: e.dma_start(out=wr[:, :, j * 128:j * 128 + 64],
                                                     in_=w_in_v[:, :, j * 64:j * 64 + 64]), writes=[wr])
            S.dma("pool", lambda e, j=j: e.dma_start(out=wr[:, :, j * 128 + 64:j * 128 + 128],
                                                     in_=w_in_v[:, :, (4 + j) * 64:(4 + j) * 64 + 64]),
                  writes=[wr])
        for c in range(8):
            for (a, b) in ((768, 2048), (2048, 3328)):
                S.dma("pool", lambda e, c=c, a=a, b=b: e.dma_start(out=wr[:, c, a - 256:b - 256],
                                                                   in_=w_in_v[:, c, a:b]), writes=[wr])
        S.dma("pool", lambda e: e.dma_start(out=wap[:], in_=w_ap.rearrange("(c p) d -> p c d", p=128)),
              writes=[wap])
        S.op("dve", lambda e: e.tensor_tensor(out=tmpb[:], in0=tmpb[:],
                                              in1=tmpm[:].unsqueeze(1).to_broadcast([128, 8, 3, 128]),
                                              op=ALU.add), reads=[tmpb, tmpm], writes=[tmpb])
        S.op("dve", lambda e: e.tensor_copy(out=bm[:], in_=tmpb[:]), reads=[tmpb], writes=[bm])
        S.op("dve", lambda e: e.tensor_tensor(out=bfst[:], in0=tmpb[:, :, 0, :],
                                              in1=tmpe[:, 0:1, :].to_broadcast([128, 8, 128]),
                                              op=ALU.add), reads=[tmpb, tmpe], writes=[bfst])
        S.op("dve", lambda e: e.tensor_tensor(out=blst[:], in0=tmpb[:, :, 2, :],
                                              in1=tmpe[:, 1:2, :].to_broadcast([128, 8, 128]),
                                              op=ALU.add), reads=[tmpb, tmpe], writes=[blst])
        S.op("act", lambda e: e.activation(out=esink[:], in_=esink[:], func=AF.Exp),
             reads=[esink], writes=[esink])
        S.op("pool", lambda e: e.memset(vaug[:, :, :, 64:65], 1.0), writes=[vaug])

        xblk_r = Rot(S, 2, [128, D], F32, "xblk")
        xn_r = Rot(S, 2, [128, D], BF16, "xn")
        junk = S.sb([128, D], BF16, "junk")
        ssq_r = Rot(S, 2, [128, 1], F32, "ssq")
        rstd_r = Rot(S, 2, [128, 1], F32, "rstd")
        hT_r = Rot(S, 2, [128, 8, 512], BF16, "hT")
        tp_r = Rot(S, 1, [128, 8, 128], BF16, "tp", psum=True)
        zp_r = Rot(S, 2, [128, 512], F32, "zp", psum=True)
        st_r = Rot(S, 2, [128, 4, 128], F32, "stp", psum=True)
        pv_r = Rot(S, 2, [128, 512], F32, "pvp", psum=True)
        qT = S.sb([128, 4, 512], BF16, "qT")
        fo_r = Rot(S, 3, [128, 512], BF16, "fo")
        sg_r = Rot(S, 3, [128, 512], F32, "sg")
        ma_r = Rot(S, 3, [128, 512], F32, "ma")
        attnT = S.sb([128, 4, 512], BF16, "attnT")
        pT_r = Rot(S, 3, [128, 3, 128], BF16, "pT")
        attn_r = Rot(S, 2, [128, 8, 64], BF16, "attn")
        den_r = Rot(S, 2, [128, 8], F32, "den")

        def norm_chunk(e0, nblk):
            hT = hT_r.next()
            for b in range(nblk):
                emit_norm_T(S, xe[e0 + b * 128:e0 + (b + 1) * 128, :], xblk_r.next(), xn_r.next(),
                            tp_r.next(), hT, hT, b * 128, g_sb, gc, ssq_r.next(), rstd_r.next(), junk)
            return hT

        for ci in range(9):
            nblk = 4 if ci < 8 else 2
            n = nblk * 128
            e0 = ci * 512
            hT = norm_chunk(e0, nblk)
            zp = zp_r.next()
            for c in range(8):
                S.op("pe", lambda e, c=c: e.matmul(zp[:, 0:n], lhsT=wkv[:, c, 0:128], rhs=hT[:, c, 0:n],
                                                   start=(c == 0), stop=(c == 7)),
                     reads=[wkv, hT], writes=[zp])
            S.op("dve", lambda e: e.tensor_copy(out=kT[:, e0:e0 + n], in_=zp[:, 0:n]),
                 reads=[zp], writes=[kT])
            for b in range(nblk):
                zp = zp_r.next()
                blk = ci * 4 + b
                for c in range(8):
                    S.op("pe", lambda e, c=c, b=b: e.matmul(zp[:, 0:128], lhsT=hT[:, c, b * 128:(b + 1) * 128],
                                                            rhs=wkv[:, c, 128:256],
                                                            start=(c == 0), stop=(c == 7)),
                         reads=[wkv, hT], writes=[zp])
                S.op("act", lambda e, blk=blk: e.activation(
                    out=vaug[:, blk, :, 0:64], in_=zp[:, 0:128].rearrange("p (k d) -> p k d", k=2),
                    func=AF.Copy), reads=[zp], writes=[vaug])

        for s in range(8):
            t0 = s * 512
            hT = norm_chunk(128 + t0, 4)
            for j in range(16):
                zp = zp_r.next()
                col = j * 128 if j < 8 else 2048 + (j - 8) * 128
                for c in range(8):
                    S.op("pe", lambda e, c=c, col=col: e.matmul(zp[:], lhsT=wr[:, c, col:col + 128],
                                                                rhs=hT[:, c, :], start=(c == 0), stop=(c == 7)),
                         reads=[wr, hT], writes=[zp])
                if j < 4:
                    S.op("dve", lambda e, j=j: e.tensor_scalar(out=qT[:, j, :], in0=zp[:], scalar1=0.125,
                                                               scalar2=None, op0=ALU.mult),
                         reads=[zp], writes=[qT])
                elif j < 8:
                    fo = fo_r.next()
                    S.op("dve", lambda e: e.tensor_copy(out=fo[:], in_=zp[:]), reads=[zp], writes=[fo])
                    S.dma("pool", lambda e, j=j: e.dma_start(out=fT[j - 4, :, t0:t0 + 512], in_=fo[:]),
                          reads=[fo])
                else:
                    sg = sg_r.next()
                    S.op("act", lambda e: e.activation(out=sg[:], in_=zp[:], func=AF.Sigmoid),
                         reads=[zp], writes=[sg])
                    S.dma("pool", lambda e, j=j: e.dma_start(out=sgbT[j - 8, :, t0:t0 + 512], in_=sg[:]),
                          reads=[sg])
            for bq in range(4):
                n = s * 4 + bq
                pv = [pv_r.next(), pv_r.next()]
                pvv = [p[:, 0:260].rearrange("p (j d) -> p j d", d=65) for p in pv]
                for half in range(2):
                    p0 = 64 * half
                    for j in range(4):
                        h = j + 4 * half
                        stp = st_r.next()
                        for kb in range(3):
                            if n == 0 and kb == 0:
                                bias_ap = bfst[:, h, :]
                                bdep = bfst
                            elif n == 31 and kb == 2:
                                bias_ap = blst[:, h, :]
                                bdep = blst
                            else:
                                bias_ap = bm[:, h, kb, :]
                                bdep = bm
                            ke = (n + kb) * 128
                            S.op("pe", lambda e, kb=kb, ke=ke, j=j: e.matmul(
                                stp[:, kb, :], lhsT=kT[p0:p0 + 64, ke:ke + 128],
                                rhs=qT[p0:p0 + 64, j, bq * 128:(bq + 1) * 128], start=True, stop=False),
                                reads=[kT, qT], writes=[stp])
                            S.op("pe", lambda e, kb=kb, bias_ap=bias_ap: e.matmul(
                                stp[:, kb, :], lhsT=ident[:], rhs=bias_ap, start=False, stop=True),
                                reads=[ident, bdep], writes=[stp])
                        pT = pT_r.next()
                        S.op("act", lambda e: e.activation(out=pT[:], in_=stp[:, 0:3, :], func=AF.Exp),
                             reads=[stp], writes=[pT])
                        for kb in range(3):
                            S.op("pe", lambda e, kb=kb, j=j: e.matmul(
                                pvv[half][:, j, :], lhsT=pT[:, kb, :], rhs=vaug[:, n + kb, half, :],
                                start=(kb == 0), stop=(kb == 2)), reads=[pT, vaug], writes=[pv[half]])
                den = den_r.next()
                attn = attn_r.next()
                for half in range(2):
                    S.op("dve", lambda e, half=half: e.tensor_tensor(
                        out=den[:, 4 * half:4 * half + 4], in0=pvv[half][:, :, 64],
                        in1=esink[:, 4 * half:4 * half + 4], op=ALU.add),
                        reads=[pv[half], esink], writes=[den])
                S.op("dve", lambda e: e.reciprocal(out=den[:], in_=den[:]), reads=[den], writes=[den])
                for half in range(2):
                    S.op("dve", lambda e, half=half: e.tensor_tensor(
                        out=attn[:, 4 * half:4 * half + 4, :], in0=pvv[half][:, :, 0:64],
                        in1=den[:, 4 * half:4 * half + 4].unsqueeze(2).to_broadcast([128, 4, 64]),
                        op=ALU.mult), reads=[pv[half], den], writes=[attn])
                tp = tp_r.next()
                for c in range(4):
                    S.op("pe", lambda e, c=c: e.transpose(
                        out=tp[:, c, :], in_=attn[:, 2 * c:2 * c + 2, :].rearrange("p a d -> p (a d)"),
                        identity=ident[:]), reads=[attn, ident], writes=[tp])
                S.op("act", lambda e: e.activation(out=attnT[:, :, bq * 128:(bq + 1) * 128],
                                                   in_=tp[:, 0:4, :], func=AF.Copy),
                     reads=[tp], writes=[attnT])
            for dc in range(8):
                zp = zp_r.next()
                for c in range(8):
                    S.op("pe", lambda e, c=c, dc=dc: e.matmul(
                        zp[:], lhsT=wr[:, c, 1024 + dc * 128:1024 + (dc + 1) * 128], rhs=hT[:, c, :],
                        start=(c == 0), stop=(c == 7)), reads=[wr, hT], writes=[zp])
                sg = sg_r.next()
                S.op("act", lambda e: e.activation(out=sg[:], in_=zp[:], func=AF.Sigmoid),
                     reads=[zp], writes=[sg])
                zp2 = zp_r.next()
                for c in range(4):
                    S.op("pe", lambda e, c=c, dc=dc: e.matmul(
                        zp2[:], lhsT=wap[:, c, dc * 128:(dc + 1) * 128], rhs=attnT[:, c, :],
                        start=(c == 0), stop=(c == 3)), reads=[wap, attnT], writes=[zp2])
                ma = ma_r.next()
                S.op("dve", lambda e: e.tensor_tensor(out=ma[:], in0=zp2[:], in1=sg[:], op=ALU.mult),
                     reads=[zp2, sg], writes=[ma])
                S.dma("pool", lambda e, dc=dc: e.dma_start(out=maT[dc, :, t0:t0 + 512], in_=ma[:]),
                      reads=[ma])
        _end(ctx, S)
        print("build_A: %d instructions" % S.ninst, S.cnt)
    return nc


_CACHE = {}


def _bucket_table():
    if "bucket" in _CACHE:
        return _CACHE["bucket"]
    import jax
    import jax.numpy as jnp
    with jax.default_device(jax.devices("cpu")[0]):
        key = jnp.arange(128)[:, None, None]
        kb = jnp.arange(3)[None, :, None]
        q = jnp.arange(128)[None, None, :]
        rel = (kb * 128 + key - 128) - q
        half = 16
        max_exact = 8
        ret = (rel > 0).astype(jnp.int32) * half
        n = jnp.abs(rel)
        nf = jnp.maximum(n, 1).astype(jnp.float32)
        large = max_exact + (jnp.log(nf / max_exact) / np.float32(np.log(128 / max_exact))
                             * (half - max_exact)).astype(jnp.int32)
        large = jnp.minimum(large, half - 1)
        bucket = np.asarray(ret + jnp.where(n < max_exact, n, large))
        rel = np.asarray(rel)
    _CACHE["bucket"] = (bucket, rel)
    return _CACHE["bucket"]


def host_inputs_A(x, l, inp):
    bucket, rel = _bucket_table()
    rel_bias = inp["rel_bias"]
    biasg = np.ascontiguousarray(rel_bias[bucket].transpose(0, 3, 1, 2))
    maskw = np.where(np.abs(rel) <= 128, 0.0, NEG).astype(np.float32)
    common = {
        "w_in": np.ascontiguousarray(inp["w_in"][l]),
        "g": np.ascontiguousarray(inp["g_mix"][l].reshape(8, 128).T),
        "biasg": biasg.astype(np.float32),
        "maskw": maskw,
        "sinkb": np.ascontiguousarray(np.broadcast_to(inp["attn_sink"][l][None, :], (128, 8))),
        "w_ap": np.ascontiguousarray(inp["w_attn_proj"][l]),
        "ident": np.eye(128, dtype=np.float32),
    }
    maps = []
    for c in range(NCORES):
        b, r = divmod(c, 4)
        xe = np.zeros((TOK + 256, D), np.float32)
        lo = r * TOK - 128
        hi = lo + TOK + 256
        slo, shi = max(lo, 0), min(hi, SEQ)
        xe[slo - lo:shi - lo] = x[b, slo:shi]
        edges = np.zeros((128, 2, 128), np.float32)
        if r == 0:
            edges[:, 0, :] = NEG
        if r == 3:
            edges[:, 1, :] = NEG
        m = dict(common)
        m["xe"] = xe
        m["edges"] = edges
        maps.append(m)
    return maps


def build_B(ctx=None, io=None):
    nc = ctx.nc if ctx is not None else bass.Bass("TRN2", target_bir_lowering=False)
    fin = None if (io is not None and "fin_parts" in io) else _D(nc, io, "fin", [128, 128, 128], BF16, "ExternalInput")
    cs_in = _D(nc, io, "cs", [128, 256], BF16, "ExternalInput")
    cc_in = _D(nc, io, "ccsc", [128, 256], BF16, "ExternalInput")
    xk_in = _D(nc, io, "xk", [128, 128, 2, 256], BF16, "ExternalInput")
    YT = _D(nc, io, "YT", [128, SEQ], BF16, "ExternalOutput")
    scale = float(1.0 / np.sqrt(SEQ * 128.0))
    with ExitStack() as st:
        S = _S(ctx, nc, st)
        f_sb = S.sb([128, 128, 128], BF16, "f_sb")
        a_sb = S.sb([128, 2, 128, 128], BF16, "a_sb")
        y_sb = S.sb([128, SEQ], BF16, "y_sb")
        cs = S.sb([128, 256], BF16, "cs_sb")
        cc = S.sb([128, 256], BF16, "cc_sb")
        xk_r = Rot(S, 3, [128, 8, 2, 256], BF16, "xk")
        gg_r = Rot(S, 2, [128, 2, 4, 128], BF16, "gg")
        p1_r = Rot(S, 2, [128, 2, 256], F32, "p1", psum=True)
        p3_r = Rot(S, 4, [128, 2, 256], F32, "p3", psum=True)
        p4_r = Rot(S, 2, [128, 512], F32, "p4", psum=True)
        for q in range(4):
            if fin is None:
                S.dma("sp", lambda e, q=q: e.dma_start(out=f_sb[q * 32:(q + 1) * 32, :, :],
                                                       in_=io["fin_parts"][q]), writes=[f_sb])
            else:
                S.dma("sp", lambda e, q=q: e.dma_start(out=f_sb[:, q * 32:(q + 1) * 32, :],
                                                       in_=fin[:, q * 32:(q + 1) * 32, :]), writes=[f_sb])
        S.dma("sp", lambda e: e.dma_start(out=cs[:], in_=cs_in), writes=[cs])
        S.dma("sp", lambda e: e.dma_start(out=cc[:], in_=cc_in), writes=[cc])
        for c2 in range(64):
            p1 = p1_r.next()
            for i in range(2):
                c = c2 * 2 + i
                S.op("pe", lambda e, c=c, i=i: e.matmul(p1[:, i, :], lhsT=f_sb[:, c, :], rhs=cs[:],
                                                        start=True, stop=True), reads=[f_sb, cs], writes=[p1])
            eng = "dve" if c2 % 2 == 0 else "act"
            src = p1[:].rearrange("p c (r k) -> p r k c", r=2)
            dst = a_sb[:, :, :, c2 * 2:c2 * 2 + 2]
            if eng == "dve":
                S.op("dve", lambda e: e.tensor_copy(out=dst, in_=src), reads=[p1], writes=[a_sb])
            else:
                S.op("act", lambda e: e.activation(out=dst, in_=src, func=AF.Copy), reads=[p1], writes=[a_sb])
        yv = y_sb[:].rearrange("p (k2 k1) -> p k1 k2", k1=128)
        for g8 in range(16):
            xk = xk_r.next()
            S.dma("sp", lambda e, g8=g8: e.dma_start(out=xk[:], in_=xk_in[:, g8 * 8:(g8 + 1) * 8, :, :]),
                  writes=[xk])
            for g4 in range(2):
                gg = gg_r.next()
                for pr in range(2):
                    p3 = p3_r.next()
                    for i in range(2):
                        kk = g4 * 4 + pr * 2 + i
                        k1 = g8 * 8 + kk
                        S.op("pe", lambda e, k1=k1, kk=kk, i=i: e.matmul(
                            p3[:, i, :], lhsT=a_sb[:, 0, k1, :], rhs=xk[:, kk, 0, :], start=True, stop=False),
                            reads=[a_sb, xk], writes=[p3])
                        S.op("pe", lambda e, k1=k1, kk=kk, i=i: e.matmul(
                            p3[:, i, :], lhsT=a_sb[:, 1, k1, :], rhs=xk[:, kk, 1, :], start=False, stop=True),
                            reads=[a_sb, xk], writes=[p3])
                    src = p3[:].rearrange("p k (r q) -> p r k q", r=2)
                    dst = gg[:, :, pr * 2:pr * 2 + 2, :]
                    if pr == 0:
                        S.op("dve", lambda e: e.tensor_copy(out=dst, in_=src), reads=[p3], writes=[gg])
                    else:
                        S.op("act", lambda e: e.activation(out=dst, in_=src, func=AF.Copy),
                             reads=[p3], writes=[gg])
                p4 = p4_r.next()
                S.op("pe", lambda e: e.matmul(p4[:], lhsT=cc[:, 0:128],
                                              rhs=gg[:, 0, :, :].rearrange("p k q -> p (k q)"),
                                              start=True, stop=False), reads=[cc, gg], writes=[p4])
                S.op("pe", lambda e: e.matmul(p4[:], lhsT=cc[:, 128:256],
                                              rhs=gg[:, 1, :, :].rearrange("p k q -> p (k q)"),
                                              start=False, stop=True), reads=[cc, gg], writes=[p4])
                k1g = g8 * 8 + g4 * 4
                S.op("dve", lambda e, k1g=k1g: e.tensor_scalar(
                    out=yv[:, k1g:k1g + 4, :], in0=p4[:].rearrange("p (k q) -> p k q", k=4),
                    scalar1=scale, scalar2=None, op0=ALU.mult), reads=[p4], writes=[y_sb])
        for q in range(4):
            S.dma("sp", lambda e, q=q: e.dma_start(out=YT[:, q * 4096:(q + 1) * 4096],
                                                   in_=y_sb[:, q * 4096:(q + 1) * 4096]), reads=[y_sb])
        _end(ctx, S)
        print("build_B: %d instructions" % S.ninst, S.cnt)
    return nc


def host_consts_B():
    if "B" in _CACHE:
        return _CACHE["B"]
    j = np.arange(128, dtype=np.float64)
    ang = 2.0 * np.pi * np.outer(j, j) / 128.0
    C, Sn = np.cos(ang), np.sin(ang)
    cs = np.concatenate([C, -Sn], axis=1).astype(NPBF)
    ccsc = np.concatenate([C, Sn], axis=1).astype(NPBF)
    n2 = np.arange(128, dtype=np.float64)[:, None, None]
    k1 = np.arange(128, dtype=np.float64)[None, :, None]
    k2 = np.arange(128, dtype=np.float64)[None, None, :]
    kk = (k1 + 128.0 * k2)
    ph = 2.0 * np.pi * ((n2 * kk) % SEQ) / SEQ
    Rc, Rs = np.cos(ph), np.sin(ph)
    xk = np.empty((128, 128, 2, 256), dtype=NPBF)
    xk[:, :, 0, 0:128] = Rc.astype(NPBF)
    xk[:, :, 0, 128:256] = (-Rs).astype(NPBF)
    xk[:, :, 1, 0:128] = Rs.astype(NPBF)
    xk[:, :, 1, 128:256] = Rc.astype(NPBF)
    _CACHE["B"] = {"cs": cs, "ccsc": ccsc, "xk": xk}
    return _CACHE["B"]


def host_inputs_B(fT_list):
    cst = host_consts_B()
    maps = []
    for c in range(NCORES):
        b, g = divmod(c, 4)
        f_bg = np.concatenate([np.asarray(fT_list[b * 4 + r])[g] for r in range(4)], axis=1)
        fin = np.ascontiguousarray(f_bg.reshape(128, 128, 128).transpose(1, 0, 2))
        m = dict(cst)
        m["fin"] = fin
        maps.append(m)
    return maps


def build_C(ctx=None, io=None):
    nc = ctx.nc if ctx is not None else bass.Bass("TRN2", target_bir_lowering=False)
    YTi = _D(nc, io, "YTi", [4, 128, TOK], BF16, "ExternalInput")
    maT = _D(nc, io, "maT", [8, 128, TOK], F32, "ExternalInput")
    sgbT = _D(nc, io, "sgbT", [8, 128, TOK], F32, "ExternalInput")
    x = _D(nc, io, "x", [TOK, D], F32, "ExternalInput")
    w_fp = _D(nc, io, "w_fp", [512, D], F32, "ExternalInput")
    w_out = _D(nc, io, "w_out", [D, D], F32, "ExternalInput")
    gb_in = _D(nc, io, "gb", [128, D], F32, "ExternalInput")
    w_rt = _D(nc, io, "w_rt", [D, NEXP], F32, "ExternalInput")
    identf_in = _D(nc, io, "identf", [128, 128], F32, "ExternalInput")
    x1o = _D(nc, io, "x1", [TOK, D], F32, "ExternalOutput")
    h2o = _D(nc, io, "h2", [TOK, D], BF16, "ExternalOutput")
    affo = _D(nc, io, "aff", [TOK, NEXP], F32, "ExternalOutput")
    affTo = _D(nc, io, "affT", [NEXP, TOK], F32, "ExternalOutput")
    with ExitStack() as st:
        S = _S(ctx, nc, st)
        wfp = S.sb([128, 4, D], BF16, "wfp")
        wout = S.sb([128, 8, D], BF16, "wout")
        gb = S.sb([128, D], F32, "gbs")
        wrt = S.sb([128, 8, NEXP], F32, "wrt")
        identf = S.sb([128, 128], F32, "identf_sb")
        S.dma("pool", lambda e: e.dma_start(out=wfp[:], in_=w_fp.rearrange("(c p) d -> p c d", p=128)),
              writes=[wfp])
        for c in range(8):
            S.dma("pool", lambda e, c=c: e.dma_start(out=wout[:, c, :], in_=w_out[c * 128:(c + 1) * 128, :]),
                  writes=[wout])
        S.dma("sp", lambda e: e.dma_start(out=gb[:], in_=gb_in), writes=[gb])
        S.dma("sp", lambda e: e.dma_start(out=wrt[:], in_=w_rt.rearrange("(c p) e -> p c e", p=128)),
              writes=[wrt])
        S.dma("sp", lambda e: e.dma_start(out=identf[:], in_=identf_in), writes=[identf])
        yt_r = Rot(S, 2, [128, 4, 512], BF16, "yt")
        sg_r = Rot(S, 2, [128, 8, 512], F32, "sgt")
        ma_r = Rot(S, 2, [128, 8, 512], F32, "mat")
        mg_r = Rot(S, 2, [128, 8, 512], BF16, "mg")
        xb_r = Rot(S, 2, [128, D], F32, "xb")
        x1_r = Rot(S, 2, [128, D], F32, "x1t")
        h2f_r = Rot(S, 2, [128, D], F32, "h2f")
        h2b_r = Rot(S, 2, [128, D], BF16, "h2b")
        h2T_r = Rot(S, 2, [128, 8, 128], F32, "h2T")
        junk = S.sb([128, D], BF16, "junkc")
        ssq_r = Rot(S, 2, [128, 1], F32, "ssqc")
        rstd_r = Rot(S, 2, [128, 1], F32, "rstdc")
        mx_r = Rot(S, 2, [128, 1], F32, "mxc")
        sm_r = Rot(S, 2, [128, 1], F32, "smc")
        ex_r = Rot(S, 2, [128, NEXP], F32, "exc")
        af_r = Rot(S, 2, [128, NEXP], F32, "afc")
        aft_r = Rot(S, 2, [NEXP, 128], F32, "aftc")
        tmp_r = Rot(S, 2, [128, 512], F32, "tmpc")
        fo_p = Rot(S, 2, [128, 512], F32, "fop", psum=True)
        op_p = Rot(S, 2, [128, 512], F32, "opp", psum=True)
        tp_p = Rot(S, 2, [128, 4, 128], F32, "tpp", psum=True)
        rt_p = Rot(S, 1, [128, 512], F32, "rtp", psum=True)

        for s in range(8):
            t0 = s * 512
            yt, sg, ma, mg = yt_r.next(), sg_r.next(), ma_r.next(), mg_r.next()
            S.dma("sp", lambda e: e.dma_start(out=yt[:], in_=YTi[:, :, t0:t0 + 512].rearrange("c p t -> p c t")),
                  writes=[yt])
            S.dma("sp", lambda e: e.dma_start(out=sg[:], in_=sgbT[:, :, t0:t0 + 512].rearrange("c p t -> p c t")),
                  writes=[sg])
            S.dma("sp", lambda e: e.dma_start(out=ma[:], in_=maT[:, :, t0:t0 + 512].rearrange("c p t -> p c t")),
                  writes=[ma])
            for dc in range(8):
                fo = fo_p.next()
                for c in range(4):
                    S.op("pe", lambda e, c=c, dc=dc: e.matmul(fo[:], lhsT=wfp[:, c, dc * 128:(dc + 1) * 128],
                                                              rhs=yt[:, c, :], start=(c == 0), stop=(c == 3)),
                         reads=[wfp, yt], writes=[fo])
                tmp = tmp_r.next()
                S.op("dve", lambda e, dc=dc: e.tensor_tensor(out=tmp[:], in0=fo[:], in1=sg[:, dc, :], op=ALU.mult),
                     reads=[fo, sg], writes=[tmp])
                S.op("pool", lambda e, dc=dc: e.tensor_tensor(out=mg[:, dc, :], in0=tmp[:], in1=ma[:, dc, :],
                                                              op=ALU.add), reads=[tmp, ma], writes=[mg])
            for b in range(4):
                r0 = t0 + b * 128
                xb, x1t, h2f, h2b = xb_r.next(), x1_r.next(), h2f_r.next(), h2b_r.next()
                S.dma("sp", lambda e: e.dma_start(out=xb[:], in_=x[r0:r0 + 128, :]), writes=[xb])
                for hf in range(2):
                    op = op_p.next()
                    for c in range(8):
                        S.op("pe", lambda e, c=c, hf=hf: e.matmul(
                            op[:], lhsT=mg[:, c, b * 128:(b + 1) * 128], rhs=wout[:, c, hf * 512:(hf + 1) * 512],
                            start=(c == 0), stop=(c == 7)), reads=[mg, wout], writes=[op])
                    S.op("dve", lambda e, hf=hf: e.tensor_tensor(out=x1t[:, hf * 512:(hf + 1) * 512], in0=op[:],
                                                                 in1=xb[:, hf * 512:(hf + 1) * 512], op=ALU.add),
                         reads=[op, xb], writes=[x1t])
                S.dma("pool", lambda e: e.dma_start(out=x1o[r0:r0 + 128, :], in_=x1t[:]), reads=[x1t])
                ssq, rstd = ssq_r.next(), rstd_r.next()
                S.op("act", lambda e: e.activation(out=junk[:], in_=x1t[:], func=AF.Square, accum_out=ssq[:, 0:1]),
                     reads=[x1t], writes=[junk, ssq])
                S.op("dve", lambda e: e.tensor_scalar(out=rstd[:, 0:1], in0=ssq[:, 0:1], scalar1=1.0 / D,
                                                      scalar2=EPS, op0=ALU.mult, op1=ALU.add),
                     reads=[ssq], writes=[rstd])
                S.op("act", lambda e: e.activation(out=rstd[:, 0:1], in_=rstd[:, 0:1], func=AF.Sqrt),
                     reads=[rstd], writes=[rstd])
                S.op("dve", lambda e: e.reciprocal(out=rstd[:, 0:1], in_=rstd[:, 0:1]), reads=[rstd], writes=[rstd])
                S.op("dve", lambda e: e.scalar_tensor_tensor(out=h2f[:], in0=x1t[:], scalar=rstd[:, 0:1], in1=gb[:],
                                                             op0=ALU.mult, op1=ALU.mult),
                     reads=[x1t, rstd, gb], writes=[h2f])
                S.op("act", lambda e: e.activation(out=h2b[:], in_=h2f[:], func=AF.Copy), reads=[h2f], writes=[h2b])
                S.dma("pool", lambda e: e.dma_start(out=h2o[r0:r0 + 128, :], in_=h2b[:]), reads=[h2b])
                h2T = h2T_r.next()
                for q4 in range(2):
                    tp = tp_p.next()
                    for i in range(4):
                        c = q4 * 4 + i
                        S.op("pe", lambda e, c=c, i=i: e.transpose(out=tp[:, i, :], in_=h2f[:, c * 128:(c + 1) * 128],
                                                                   identity=identf[:]),
                             reads=[h2f, identf], writes=[tp])
                    S.op("dve", lambda e, q4=q4: e.tensor_copy(out=h2T[:, q4 * 4:(q4 + 1) * 4, :], in_=tp[:]),
                         reads=[tp], writes=[h2T])
                rt = rt_p.next()
                for c in range(8):
                    S.op("pe", lambda e, c=c: e.matmul(rt[:, 0:NEXP], lhsT=h2T[:, c, :], rhs=wrt[:, c, :],
                                                       start=(c == 0), stop=(c == 7)), reads=[h2T, wrt], writes=[rt])
                mx, sm, ex, af = mx_r.next(), sm_r.next(), ex_r.next(), af_r.next()
                S.op("dve", lambda e: e.reduce_max(out=mx[:, 0:1], in_=rt[:, 0:NEXP], axis=AX.X, negate=True),
                     reads=[rt], writes=[mx])
                S.op("act", lambda e: e.activation(out=ex[:], in_=rt[:, 0:NEXP], func=AF.Exp, bias=mx[:, 0:1],
                                                   accum_out=sm[:, 0:1]), reads=[rt, mx], writes=[ex, sm])
                S.op("dve", lambda e: e.reciprocal(out=sm[:, 0:1], in_=sm[:, 0:1]), reads=[sm], writes=[sm])
                S.op("dve", lambda e: e.tensor_scalar(out=af[:], in0=ex[:], scalar1=sm[:, 0:1], scalar2=None,
                                                      op0=ALU.mult), reads=[ex, sm], writes=[af])
                S.dma("pool", lambda e: e.dma_start(out=affo[r0:r0 + 128, :], in_=af[:]), reads=[af])
                tpa = tp_p.next()
                aft = aft_r.next()
                S.op("pe", lambda e: e.transpose(out=tpa[0:NEXP, 0, :], in_=af[:], identity=identf[:]),
                     reads=[af, identf], writes=[tpa])
                S.op("dve", lambda e: e.tensor_copy(out=aft[:], in_=tpa[0:NEXP, 0, :]), reads=[tpa], writes=[aft])
                S.dma("pool", lambda e: e.dma_start(out=affTo[:, r0:r0 + 128], in_=aft[:]), reads=[aft])
        _end(ctx, S)
        print("build_C: %d instructions" % S.ninst, S.cnt)
    return nc


def host_inputs_C(x, l, inp, YT_list, maT_list, sgbT_list):
    common = {
        "w_fp": np.ascontiguousarray(inp["w_fourier_proj"][l]),
        "w_out": np.ascontiguousarray(inp["w_out"][l]),
        "gb": np.ascontiguousarray(np.broadcast_to(inp["g_ffn"][l][None, :], (128, D))),
        "w_rt": np.ascontiguousarray(inp["w_router"][l]),
        "identf": np.eye(128, dtype=np.float32),
    }
    maps = []
    for c in range(NCORES):
        b, r = divmod(c, 4)
        yti = np.stack([np.asarray(YT_list[b * 4 + g])[:, r * TOK:(r + 1) * TOK] for g in range(4)], axis=0)
        m = dict(common)
        m["YTi"] = np.ascontiguousarray(yti)
        m["maT"] = np.asarray(maT_list[c])
        m["sgbT"] = np.asarray(sgbT_list[c])
        m["x"] = np.ascontiguousarray(x[b, r * TOK:(r + 1) * TOK])
        maps.append(m)
    return maps


NBIS = 36
FG = [(2 * k, 2) for k in range(11)]


def build_E(n_exp=2, n_b=NB, ctx=None, io=None):
    nc = ctx.nc if ctx is not None else bass.Bass("TRN2", target_bir_lowering=False)
    affT = _D(nc, io, "affT", [4, SEQ], F32, "ExternalInput")
    assert n_exp * n_b == 4
    h2 = io["h2"] if io is not None else [
        nc.dram_tensor("h2_%d" % b, [SEQ, D], BF16, kind="ExternalInput").ap() for b in range(n_b)]
    wg = _D(nc, io, "wg", [n_exp, D, DFF], F32, "ExternalInput")
    wu = _D(nc, io, "wu", [n_exp, D, DFF], F32, "ExternalInput")
    wd = _D(nc, io, "wd", [n_exp, DFF, D], F32, "ExternalInput")
    identf_in = _D(nc, io, "identf", [128, 128], F32, "ExternalInput")
    ustrict_in = _D(nc, io, "ustrict", [128, 128], F32, "ExternalInput")
    bones_in = _D(nc, io, "bones", [128, 128], F32, "ExternalInput")
    tokid_in = _D(nc, io, "tokid", [128, 128], F32, "ExternalInput")
    ident_in = _D(nc, io, "ident", [128, 128], F32, "ExternalInput")
    if io is not None:
        yo = io["y"]
        lst = io["lst"]
    else:
        yfull = nc.dram_tensor("y", [4, CAP + 1, D], BF16, kind="ExternalOutput").ap()
        yo = [yfull[p] for p in range(4)]
        lst = [nc.dram_tensor("lst%d" % p, [CAP + 128, 2], F32, kind="ExternalOutput").ap() for p in range(4)]
    invo = _D(nc, io, "inv", [4, SEQ], I32, "ExternalOutput")
    invTo = _D(nc, io, "invT", [4, 128, 128], I32, "ExternalOutput")
    wcache = _D(nc, io, "wcache", [11, 2, 128, 8 * 256], BF16, "Internal")
    pidx_in = _D(nc, io, "pidx", [128, 1], F32, "ExternalInput")
    thr = _D(nc, io, "thr", [128, 1], F32, "ExternalOutput")
    with ExitStack() as st:
        S = _S(ctx, nc, st)
        ustrict = S.sb([128, 128], F32, "ustrict_sb")
        bones = S.sb([128, 128], F32, "bones_sb")
        ident = S.sb([128, 128], BF16, "ident_sb")
        ones = S.sb([128, 128], F32, "ones_sb")
        identf = S.sb([128, 128], F32, "identf_sb")
        S.dma("sp", lambda e: e.dma_start(out=identf[:], in_=identf_in), writes=[identf])
        zrow = S.sb([1, D], BF16, "zrow")
        abis = S.sb([128, 512], F32, "abis")
        junk = S.sb([128, 512], F32, "junke")
        lo = S.sb([128, 1], F32, "lo")
        hi = S.sb([128, 1], F32, "hi")
        mid = S.sb([128, 1], F32, "mid")
        cp = S.sb([128, 1], F32, "cp")
        ge = S.sb([128, 1], F32, "ge")
        t2 = S.sb([128, 1], F32, "t2")
        thr_all = S.sb([128, 4], F32, "thr_all")
        cnt_p = Rot(S, 1, [128, 512], F32, "cntp", psum=True)
        thr_buf = Buf(None, "thr_d")
        lst_buf = [Buf(None, "lst%d" % p) for p in range(4)]

        S.dma("sp", lambda e: e.dma_start(out=ustrict[:], in_=ustrict_in), writes=[ustrict])
        S.dma("sp", lambda e: e.dma_start(out=bones[:], in_=bones_in), writes=[bones])
        S.dma("pool", lambda e: e.dma_start(out=ident[:], in_=ident_in), writes=[ident])
        S.dma("sp", lambda e: e.dma_start(out=abis[:], in_=affT.rearrange("p (r c) -> (p r) c", c=512)),
              writes=[abis])
        S.op("dve", lambda e: e.memset(ones[:], 1.0), writes=[ones])
        S.op("dve", lambda e: e.memset(zrow[:], 0.0), writes=[zrow])
        S.op("dve", lambda e: e.memset(lo[:], 0.0), writes=[lo])
        S.op("dve", lambda e: e.memset(hi[:], 1.0), writes=[hi])
        for p in range(4):
            S.dma("sp", lambda e, p=p: e.dma_start(out=yo[p][CAP:CAP + 1, :], in_=zrow[:]), reads=[zrow])
        for it in range(NBIS):
            cps = cnt_p.next()
            S.op("dve", lambda e: e.tensor_tensor(out=mid[:], in0=lo[:], in1=hi[:], op=ALU.add),
                 reads=[lo, hi], writes=[mid])
            S.op("dve", lambda e: e.tensor_scalar(out=mid[:], in0=mid[:], scalar1=0.5, scalar2=None, op0=ALU.mult),
                 reads=[mid], writes=[mid])
            S.op("dve", lambda e: e.tensor_scalar(out=junk[:], in0=abis[:], scalar1=mid[:, 0:1], scalar2=0.0,
                                                  op0=ALU.is_ge, op1=ALU.add, accum_out=cp[:, 0:1]),
                 reads=[abis, mid], writes=[junk, cp])
            S.op("pe", lambda e: e.matmul(cps[:, 0:1], lhsT=bones[:], rhs=cp[:, 0:1], start=True, stop=True),
                 reads=[bones, cp], writes=[cps])
            S.op("dve", lambda e: e.tensor_scalar(out=ge[:], in0=cps[:, 0:1], scalar1=float(CAP) - 0.5, scalar2=None,
                                                  op0=ALU.is_ge), reads=[cps], writes=[ge])
            S.op("dve", lambda e: e.scalar_tensor_tensor(out=lo[:], in0=mid[:], scalar=ge[:, 0:1], in1=lo[:],
                                                         op0=ALU.mult, op1=ALU.max),
                 reads=[mid, ge, lo], writes=[lo])
            S.op("dve", lambda e: e.scalar_tensor_tensor(out=t2[:], in0=ge[:], scalar=2.0, in1=mid[:],
                                                         op0=ALU.mult, op1=ALU.add),
                 reads=[mid, ge], writes=[t2])
            S.op("dve", lambda e: e.tensor_tensor(out=hi[:], in0=hi[:], in1=t2[:], op=ALU.min),
                 reads=[hi, t2], writes=[hi])
        S.dma("sp", lambda e: e.dma_start(out=thr, in_=lo[:]), reads=[lo], writes=[thr_buf])
        S.dma("sp", lambda e: e.dma_start(
            out=thr_all[:], in_=thr.rearrange("(p r) o -> r (p o)", r=32)[0, :].partition_broadcast(128),
            allow_slow_non_contiguous=True),
            reads=[thr_buf], writes=[thr_all])

        atok = S.sb([128, 128], F32, "atok")
        mask = S.sb([128, 128], F32, "mask")
        csum = S.sb([128, 128], F32, "csum")
        posf = S.sb([128, 128], F32, "posf")
        offs = S.sb([128, 1], F32, "offs")
        pay = S.sb([128, 128, 2], F32, "pay")
        invi = S.sb([128, 128], I32, "invi")
        invTi = S.sb([128, 128], I32, "invTi")
        lsb2 = [S.sb([128, 16, 2], F32, "lsb%d" % k) for k in range(2)]
        idsi2 = [S.sb([128, 16], I32, "idsi%d" % k) for k in range(2)]
        gates2 = [S.sb([128, 16], F32, "gates%d" % k) for k in range(2)]
        xs4 = [S.sb([128, D], BF16, "xs%d" % k) for k in range(4)]
        xsT_r = Rot(S, 2, [128, 8, 512], BF16, "xsT")
        actT = S.sb([128, 22, 512], BF16, "actT")
        wd_r = Rot(S, 2, [128, 22, D], BF16, "wd_sb")
        wg_r = Rot(S, 4, [128, 8, 256], BF16, "wg_t")
        wu_r = Rot(S, 4, [128, 8, 256], BF16, "wu_t")
        sl_r = Rot(S, 2, [128, 512], F32, "sl")
        yt_r = Rot(S, 2, [128, D], BF16, "yt")
        tp_p = Rot(S, 1, [128, 8, 128], BF16, "tpe", psum=True)
        hg_p = Rot(S, 2, [128, 512], F32, "hgp", psum=True)
        hu_p = Rot(S, 2, [128, 512], F32, "hup", psum=True)
        yp_p = Rot(S, 2, [128, 512], F32, "ypp", psum=True)
        S.dma("sp", lambda e: e.dma_start(out=pay[:, :, 0], in_=tokid_in, allow_slow_non_contiguous=True), writes=[pay])
        pidx = S.sb([128, 1], F32, "pidx_sb")
        sidx = S.sb([128, 128], F32, "sidx")
        sidi = S.sb([128, 128], I32, "sidi")
        S.dma("sp", lambda e: e.dma_start(out=pidx[:], in_=pidx_in), writes=[pidx])
        problems = [(ei, b) for ei in range(n_exp) for b in range(n_b)]
        wc_buf = [[Buf(None, "wc%d_%d" % (f, k)) for k in range(2)] for f in range(11)]

        def s1_chunks(p):
            par = p % 2
            lsb, idsi, gates = lsb2[par], idsi2[par], gates2[par]

            def prep():
                S.dma("sp", lambda e: e.dma_start(out=atok[:], in_=affT[p].rearrange("(i j) -> i j", j=128)),
                      writes=[atok])
                S.op("dve", lambda e: e.tensor_scalar(out=mask[:], in0=atok[:], scalar1=thr_all[:, p:p + 1],
                                                      scalar2=None, op0=ALU.is_ge),
                     reads=[atok, thr_all], writes=[mask])
                S.op("dve", lambda e: e.tensor_tensor_scan(out=csum[:], data0=ones[:], data1=mask[:], initial=0.0,
                                                           op0=ALU.mult, op1=ALU.add),
                     reads=[ones, mask], writes=[csum])
                cps = cnt_p.next()
                S.op("pe", lambda e: e.matmul(cps[:, 0:1], lhsT=ustrict[:], rhs=csum[:, 127:128], start=True, stop=True),
                     reads=[ustrict, csum], writes=[cps])
                S.op("dve", lambda e: e.tensor_copy(out=offs[:], in_=cps[:, 0:1]), reads=[cps], writes=[offs])
                S.op("dve", lambda e: e.tensor_scalar(out=posf[:], in0=csum[:], scalar1=offs[:, 0:1],
                                                      scalar2=-1.0 - CAP, op0=ALU.add, op1=ALU.add),
                     reads=[csum, offs], writes=[posf])
                S.op("dve", lambda e: e.tensor_tensor(out=posf[:], in0=posf[:], in1=mask[:], op=ALU.mult),
                     reads=[posf, mask], writes=[posf])
                S.op("dve", lambda e: e.tensor_scalar(out=posf[:], in0=posf[:], scalar1=float(CAP), scalar2=float(CAP),
                                                      op0=ALU.add, op1=ALU.min), reads=[posf], writes=[posf])
                S.op("dve", lambda e: e.tensor_copy(out=invi[:], in_=posf[:]), reads=[posf], writes=[invi])
                S.op("dve", lambda e: e.tensor_scalar(out=sidx[:], in0=mask[:], scalar1=-1.0, scalar2=1.0,
                                                      op0=ALU.mult, op1=ALU.add), reads=[mask], writes=[sidx])
                S.op("dve", lambda e: e.scalar_tensor_tensor(out=sidx[:], in0=sidx[:], scalar=pidx[:, 0:1], in1=posf[:],
                                                             op0=ALU.mult, op1=ALU.add),
                     reads=[sidx, pidx, posf], writes=[sidx])
                S.op("dve", lambda e: e.tensor_copy(out=sidi[:], in_=sidx[:]), reads=[sidx], writes=[sidi])
                S.op("dve", lambda e: e.tensor_copy(out=pay[:, :, 1], in_=atok[:]), reads=[atok], writes=[pay])
                S.dma("sp", lambda e: e.dma_start(out=invo[p].rearrange("(i j) -> i j", j=128), in_=invi[:]),
                      reads=[invi])
                cpt = cnt_p.next()
                S.op("pe", lambda e: e.transpose(out=cpt[:, 0:128], in_=posf[:], identity=identf[:]),
                     reads=[posf, identf], writes=[cpt])
                S.op("dve", lambda e: e.tensor_copy(out=invTi[:], in_=cpt[:, 0:128]), reads=[cpt], writes=[invTi])
                S.dma("sp", lambda e: e.dma_start(out=invTo[p], in_=invTi[:]), reads=[invTi])

            def scat(k):
                for j in range(16 * k, 16 * k + 16):
                    S.dma("pool", lambda e, j=j: e.indirect_dma_start(
                        out=lst[p], out_offset=bass.IndirectOffsetOnAxis(ap=sidi[:, j:j + 1], axis=0),
                        in_=pay[:, j, :], in_offset=None), reads=[sidi, pay], also_writes=[lst_buf[p]])

            def fin():
                S.dma("sp", lambda e: e.dma_start(out=lsb[:], in_=lst[p][0:CAP, :].rearrange("(t s) two -> s t two", s=128)),
                      reads=[lst_buf[p]], writes=[lsb])
                S.op("dve", lambda e: e.tensor_copy(out=idsi[:], in_=lsb[:, :, 0]), reads=[lsb], writes=[idsi])
                S.op("dve", lambda e: e.tensor_copy(out=gates[:], in_=lsb[:, :, 1]), reads=[lsb], writes=[gates])
            return [prep] + [(lambda k=k: scat(k)) for k in range(8)] + [fin]

        def emit_gathers(pp, sgg):
            bb = problems[pp][1]
            ids_ = idsi2[pp % 2]
            for t4 in range(4):
                t = sgg * 4 + t4
                S.dma("pool", lambda e, t=t, t4=t4: e.indirect_dma_start(
                    out=xs4[t4][:], out_offset=None, in_=h2[bb],
                    in_offset=bass.IndirectOffsetOnAxis(ap=ids_[:, t:t + 1], axis=0)),
                    reads=[ids_], writes=[xs4[t4]])

        def wd_load(ei, wd_sb, lo, hi):
            for c in range(lo, hi):
                S.dma("pool", lambda e, c=c: e.dma_start(out=wd_sb[:, c, :], in_=wd[ei, c * 128:(c + 1) * 128, :]),
                      writes=[wd_sb])

        wd_cur = wd_r.next()
        wd_load(problems[0][0], wd_cur, 0, 22)
        for th in s1_chunks(0):
            th()
        for p, (ei, b) in enumerate(problems):
            par = p % 2
            idsi, gates = idsi2[par], gates2[par]
            inter = []
            wd_inter = []
            if p + 1 < len(problems):
                inter = s1_chunks(p + 1)
                if problems[p + 1][0] != ei:
                    wd_nxt = wd_r.next()
                    for (lo_, hi_) in ((0, 6), (6, 12), (12, 17), (17, 22)):
                        wd_inter.append(lambda wd_nxt=wd_nxt, ne=problems[p + 1][0], lo_=lo_, hi_=hi_: wd_load(ne, wd_nxt, lo_, hi_))
                else:
                    wd_nxt = wd_cur
            slot = 0
            if p == 0:
                emit_gathers(0, 0)
            for sg in range(4):
                xsT = xsT_r.next()
                for t4 in range(4):
                    t = sg * 4 + t4
                    xs = xs4[t4]
                    tp = tp_p.next()
                    for c in range(8):
                        S.op("pe", lambda e, c=c: e.transpose(out=tp[:, c, :], in_=xs[:, c * 128:(c + 1) * 128],
                                                              identity=ident[:]), reads=[xs, ident], writes=[tp],
                             inc=(c == 7))
                    S.op("dve", lambda e, t4=t4: e.tensor_copy(out=xsT[:, :, t4 * 128:(t4 + 1) * 128], in_=tp[:]),
                         reads=[tp], writes=[xsT])
                for (c0, ncn) in FG:
                    wgt, wut = wg_r.next(), wu_r.next()
                    f0, fn = c0 * 128, ncn * 128
                    fgi = c0 // 2
                    if sg == 0 and (p == 0 or problems[p - 1][0] != ei):
                        S.dma("pool", lambda e: e.dma_start(
                            out=wgt[:, :, 0:fn], in_=wg[ei, :, f0:f0 + fn].rearrange("(c q) f -> q c f", q=128)),
                            writes=[wgt])
                        S.dma("pool", lambda e: e.dma_start(
                            out=wut[:, :, 0:fn], in_=wu[ei, :, f0:f0 + fn].rearrange("(c q) f -> q c f", q=128)),
                            writes=[wut])
                        S.dma("sp", lambda e, fgi=fgi: e.dma_start(
                            out=wcache[fgi, 0], in_=wgt[:].rearrange("q c f -> q (c f)")),
                            reads=[wgt], writes=[wc_buf[fgi][0]])
                        S.dma("sp", lambda e, fgi=fgi: e.dma_start(
                            out=wcache[fgi, 1], in_=wut[:].rearrange("q c f -> q (c f)")),
                            reads=[wut], writes=[wc_buf[fgi][1]])
                    else:
                        S.dma("sp", lambda e, fgi=fgi: e.dma_start(
                            out=wgt[:].rearrange("q c f -> q (c f)"), in_=wcache[fgi, 0]),
                            reads=[wc_buf[fgi][0]], writes=[wgt])
                        S.dma("sp", lambda e, fgi=fgi: e.dma_start(
                            out=wut[:].rearrange("q c f -> q (c f)"), in_=wcache[fgi, 1]),
                            reads=[wc_buf[fgi][1]], writes=[wut])
                    if slot % 11 == 1 and wd_inter:
                        wd_inter.pop(0)()
                    slot += 1
                    for fc in range(ncn):
                        hg, hu = hg_p.next(), hu_p.next()
                        for c in range(8):
                            S.op("pe", lambda e, c=c, fc=fc: e.matmul(
                                hg[:], lhsT=wgt[:, c, fc * 128:(fc + 1) * 128], rhs=xsT[:, c, :],
                                start=(c == 0), stop=(c == 7)), reads=[wgt, xsT], writes=[hg], inc=(c == 7))
                        for c in range(8):
                            S.op("pe", lambda e, c=c, fc=fc: e.matmul(
                                hu[:], lhsT=wut[:, c, fc * 128:(fc + 1) * 128], rhs=xsT[:, c, :],
                                start=(c == 0), stop=(c == 7)), reads=[wut, xsT], writes=[hu], inc=(c == 7))
                        sl = sl_r.next()
                        S.op("act", lambda e: e.activation(out=sl[:], in_=hg[:], func=AF.Silu),
                             reads=[hg], writes=[sl])
                        S.op("dve", lambda e, fc=fc, c0=c0: e.tensor_tensor(
                            out=actT[:, c0 + fc, :], in0=hu[:], in1=sl[:], op=ALU.mult),
                            reads=[hu, sl], writes=[actT])
                npop = {0: 3, 1: 2, 2: 2, 3: 3}[sg]
                if sg < 3:
                    emit_gathers(p, sg + 1)
                for _ in range(npop):
                    if inter:
                        inter.pop(0)()
                if sg == 3 and p + 1 < len(problems):
                    emit_gathers(p + 1, 0)
                for t4 in range(4):
                    t = sg * 4 + t4
                    yt = yt_r.next()
                    for hf in range(2):
                        yp = yp_p.next()
                        for fcc in range(22):
                            S.op("pe", lambda e, fcc=fcc, hf=hf, t4=t4: e.matmul(
                                yp[:], lhsT=actT[:, fcc, t4 * 128:(t4 + 1) * 128],
                                rhs=wd_cur[:, fcc, hf * 512:(hf + 1) * 512],
                                start=(fcc == 0), stop=(fcc == 21)), reads=[actT, wd_cur], writes=[yp],
                                inc=(fcc == 21))
                        S.op("act", lambda e, hf=hf, t=t: e.activation(
                            out=yt[:, hf * 512:(hf + 1) * 512], in_=yp[:], func=AF.Copy, scale=gates[:, t:t + 1]),
                            reads=[yp, gates], writes=[yt])
                    S.dma("sp", lambda e, t=t: e.dma_start(out=yo[p][t * 128:(t + 1) * 128, :], in_=yt[:]),
                          reads=[yt])
            while wd_inter:
                wd_inter.pop(0)()
            while inter:
                inter.pop(0)()
            if p + 1 < len(problems):
                wd_cur = wd_nxt
        _end(ctx, S)
        print("build_E: %d instructions" % S.ninst, S.cnt)
    return nc


def host_inputs_E(l, inp, affT_list, h2_list):
    affT = np.stack([np.concatenate([np.asarray(affT_list[b * 4 + r]) for r in range(4)], axis=1) for b in range(NB)])
    h2 = np.stack([np.concatenate([np.asarray(h2_list[b * 4 + r]) for r in range(4)], axis=0) for b in range(NB)])
    i = np.arange(128)
    common = {
        "h2_0": h2[0], "h2_1": h2[1],
        "ustrict": (i[:, None] < i[None, :]).astype(np.float32),
        "bones": ((i[:, None] // 32) == (i[None, :] // 32)).astype(np.float32),
        "tokid": (i[:, None] * 128 + i[None, :]).astype(np.float32),
        "pidx": i[:, None].astype(np.float32),
        "ident": np.eye(128, dtype=np.float32),
        "identf": np.eye(128, dtype=np.float32),
    }
    maps = []
    for c in range(NCORES):
        m = dict(common)
        m["affT"] = np.ascontiguousarray(np.stack([affT[b, 2 * c + ei] for ei in range(2) for b in range(NB)]))
        m["wg"] = np.ascontiguousarray(inp["w_exp_gate"][l, 2 * c:2 * c + 2])
        m["wu"] = np.ascontiguousarray(inp["w_exp_up"][l, 2 * c:2 * c + 2])
        m["wd"] = np.ascontiguousarray(inp["w_exp_down"][l, 2 * c:2 * c + 2])
        maps.append(m)
    return maps


def build_F(final, ctx=None, io=None):
    nc = ctx.nc if ctx is not None else bass.Bass("TRN2", target_bir_lowering=False)
    x1 = _D(nc, io, "x1", [TOK, D], F32, "ExternalInput")
    invT = _D(nc, io, "invT", [NEXP, 128, 32], I32, "ExternalInput")
    ys = io["ys"] if io is not None else [
        nc.dram_tensor("y%d" % e, [CAP + 1, D], BF16, kind="ExternalInput").ap() for e in range(NEXP)]
    ident_in = _D(nc, io, "ident", [128, 128], F32, "ExternalInput")
    gb_in = _D(nc, io, "gb", [128, D], F32, "ExternalInput")
    out = _D(nc, io, "out", [TOK, D], F32, "ExternalOutput")
    with ExitStack() as st:
        S = _S(ctx, nc, st)
        ident = S.sb([128, 128], BF16, "ident_sb")
        gb = S.sb([128, D], F32, "gb_sb")
        inv_sb = S.sb([128, NEXP, 32], I32, "inv_sb")
        S.dma("pool", lambda e: e.dma_start(out=ident[:], in_=ident_in), writes=[ident])
        S.dma("sp", lambda e: e.dma_start(out=gb[:], in_=gb_in), writes=[gb])
        for ex in range(NEXP):
            S.dma("sp", lambda e, ex=ex: e.dma_start(out=inv_sb[:, ex, :], in_=invT[ex]), writes=[inv_sb])
        stg_r = Rot(S, 12, [128, D], BF16, "stg")
        xb_r = Rot(S, 2, [128, D], F32, "xbf")
        x2_r = Rot(S, 2, [128, D], F32, "x2f")
        o_r = Rot(S, 2, [128, D], F32, "of")
        junk = S.sb([128, D], BF16, "junkf")
        ssq_r = Rot(S, 2, [128, 1], F32, "ssqf")
        rstd_r = Rot(S, 2, [128, 1], F32, "rstdf")
        acc_p = Rot(S, 4, [128, 512], F32, "accp", psum=True)
        for t in range(32):
            r0 = t * 128
            xb, x2 = xb_r.next(), x2_r.next()
            S.dma("sp", lambda e: e.dma_start(out=xb[:], in_=x1[r0:r0 + 128, :]), writes=[xb])
            acc = [acc_p.next(), acc_p.next()]
            for ex in range(NEXP):
                stg = stg_r.next()
                S.dma("pool", lambda e, ex=ex: e.indirect_dma_start(
                    out=stg[:], out_offset=None, in_=ys[ex],
                    in_offset=bass.IndirectOffsetOnAxis(ap=inv_sb[:, ex, t:t + 1], axis=0)),
                    reads=[inv_sb], writes=[stg])
                for hf in range(2):
                    S.op("pe", lambda e, hf=hf, ex=ex: e.matmul(acc[hf][:], lhsT=ident[:],
                                                                rhs=stg[:, hf * 512:(hf + 1) * 512],
                                                                start=(ex == 0), stop=(ex == NEXP - 1)),
                         reads=[ident, stg], writes=[acc[hf]])
            for hf in range(2):
                S.op("dve", lambda e, hf=hf: e.tensor_tensor(out=x2[:, hf * 512:(hf + 1) * 512], in0=acc[hf][:],
                                                             in1=xb[:, hf * 512:(hf + 1) * 512], op=ALU.add),
                     reads=[acc[hf], xb], writes=[x2])
            if not final:
                S.dma("sp", lambda e: e.dma_start(out=out[r0:r0 + 128, :], in_=x2[:]), reads=[x2])
            else:
                ssq, rstd, o = ssq_r.next(), rstd_r.next(), o_r.next()
                S.op("act", lambda e: e.activation(out=junk[:], in_=x2[:], func=AF.Square, accum_out=ssq[:, 0:1]),
                     reads=[x2], writes=[junk, ssq])
                S.op("dve", lambda e: e.tensor_scalar(out=rstd[:, 0:1], in0=ssq[:, 0:1], scalar1=1.0 / D,
                                                      scalar2=EPS, op0=ALU.mult, op1=ALU.add),
                     reads=[ssq], writes=[rstd])
                S.op("act", lambda e: e.activation(out=rstd[:, 0:1], in_=rstd[:, 0:1], func=AF.Sqrt),
                     reads=[rstd], writes=[rstd])
                S.op("dve", lambda e: e.reciprocal(out=rstd[:, 0:1], in_=rstd[:, 0:1]), reads=[rstd], writes=[rstd])
                S.op("dve", lambda e: e.scalar_tensor_tensor(out=o[:], in0=x2[:], scalar=rstd[:, 0:1], in1=gb[:],
                                                             op0=ALU.mult, op1=ALU.mult),
                     reads=[x2, rstd, gb], writes=[o])
                S.dma("sp", lambda e: e.dma_start(out=out[r0:r0 + 128, :], in_=o[:]), reads=[o])
        _end(ctx, S)
        print("build_F: %d instructions" % S.ninst, S.cnt)
    return nc


def host_inputs_F(inp, x1_list, y_list, invT_list):
    gb = np.ascontiguousarray(np.broadcast_to(inp["g_final"][None, :], (128, D)))
    ident = np.eye(128, dtype=np.float32)
    maps = []
    for c in range(NCORES):
        b, r = divmod(c, 4)
        m = {"x1": np.asarray(x1_list[c]), "ident": ident, "gb": gb}
        invT = np.empty((NEXP, 128, 32), np.int32)
        for ex in range(NEXP):
            cc, ei = divmod(ex, 2)
            p = ei * 2 + b
            m["y%d" % ex] = np.ascontiguousarray(np.asarray(y_list[cc])[p])
            invT[ex] = np.asarray(invT_list[cc])[p][:, r * 32:(r + 1) * 32]
        m["invT"] = invT
        maps.append(m)
    return maps


def _run(nc, maps):
    res = run_bass_kernel_spmd(nc, maps, core_ids=list(range(NCORES)))
    return res.results


def kernel_unfused(**inputs):
    inp = {k: np.asarray(v) for k, v in inputs.items()}
    x = np.ascontiguousarray(inp["x"], dtype=np.float32)
    for l in range(DEPTH):
        rA = _run(build_A(), host_inputs_A(x, l, inp))
        rB = _run(build_B(), host_inputs_B([r["fT"] for r in rA]))
        rC = _run(build_C(), host_inputs_C(x, l, inp, [r["YT"] for r in rB], [r["maT"] for r in rA],
                                           [r["sgbT"] for r in rA]))
        del rA, rB
        rE = _run(build_E(), host_inputs_E(l, inp, [r["affT"] for r in rC], [r["h2"] for r in rC]))
        rF = _run(build_F(l == DEPTH - 1), host_inputs_F(inp, [r["x1"] for r in rC], [r["y"] for r in rE],
                                                          [r["invT"] for r in rE]))
        del rC, rE
        x = np.stack([np.concatenate([np.asarray(rF[b * 4 + r]["out"]) for r in range(4)], axis=0)
                      for b in range(NB)])
    return x.astype(np.float32)


def build_fused(depth=DEPTH):
    nc = bass.Bass("TRN2", target_bir_lowering=False)
    ctx = Ctx()
    ctx.nc = nc
    EI, IN, EO = "ExternalInput", "Internal", "ExternalOutput"

    def dt_(name, shape, dt, kind):
        return nc.dram_tensor(name, list(shape), dt, kind=kind).ap()
    xpad = dt_("xpad", [SEQ + 256, D], F32, EI)
    cst = {
        "biasg": dt_("biasg", [128, 8, 3, 128], F32, EI),
        "maskw": dt_("maskw", [128, 3, 128], F32, EI),
        "ident": dt_("ident", [128, 128], F32, EI),
        "identf": dt_("identf", [128, 128], F32, EI),
        "cs": dt_("cs", [128, 256], BF16, EI),
        "ccsc": dt_("ccsc", [128, 256], BF16, EI),
        "xk": dt_("xk", [128, 128, 2, 256], BF16, EI),
        "ustrict": dt_("ustrict", [128, 128], F32, EI),
        "bones": dt_("bones", [128, 128], F32, EI),
        "tokid": dt_("tokid", [128, 128], F32, EI),
        "pidx": dt_("pidx", [128, 1], F32, EI),
    }
    edges3 = dt_("edges3", [3, 128, 2, 128], F32, EI)
    w_in = dt_("w_in", [depth, D, INW], F32, EI)
    g_mix = dt_("g_mix", [depth, 128, 8], F32, EI)
    sinkb = dt_("sinkb", [depth, 128, 8], F32, EI)
    w_ap = dt_("w_ap", [depth, 512, D], F32, EI)
    w_fp = dt_("w_fp", [depth, 512, D], F32, EI)
    w_out = dt_("w_out", [depth, D, D], F32, EI)
    gffn = dt_("gffn", [depth, 128, D], F32, EI)
    w_rt = dt_("w_rt", [depth, D, NEXP], F32, EI)
    wg = dt_("wg", [depth, NEXP, D, DFF], F32, EI)
    wu = dt_("wu", [depth, NEXP, D, DFF], F32, EI)
    wd = dt_("wd", [depth, NEXP, DFF, D], F32, EI)
    gfin = dt_("gfin", [128, D], F32, EI)
    out = dt_("out", [SEQ, D], F32, EO)
    fTs = dt_("s_fT", [4, 4, 128, TOK], BF16, IN)
    maTs = dt_("s_maT", [4, 8, 128, TOK], F32, IN)
    sgbTs = dt_("s_sgbT", [4, 8, 128, TOK], F32, IN)
    YTs = dt_("s_YT", [4, 128, SEQ], BF16, IN)
    x1s = dt_("s_x1", [SEQ, D], F32, IN)
    h2s = dt_("s_h2", [SEQ, D], BF16, IN)
    affs = dt_("s_aff", [SEQ, NEXP], F32, IN)
    affTs = dt_("s_affT", [NEXP, SEQ], F32, IN)
    ys = [dt_("s_y%d" % e, [CAP + 1, D], BF16, IN) for e in range(NEXP)]
    invs = dt_("s_inv", [4, SEQ], I32, IN)
    invTs = dt_("s_invT", [NEXP, 128, 128], I32, IN)
    lsts = [dt_("s_lst%d" % p, [CAP + 128, 2], F32, IN) for p in range(4)]
    thr = dt_("s_thr", [128, 1], F32, IN)
    xpad2 = dt_("s_xpad2", [SEQ + 256, D], F32, IN)
    wcache = dt_("s_wcache", [11, 2, 128, 8 * 256], BF16, IN)

    with ExitStack() as outer:
        S = Sched(nc, outer)
        ctx.S = S
        if depth > 1:
            with ExitStack() as st:
                S.stack = st
                S.prefix = "pz_"
                z = S.sb([128, D], F32, "zpad")
                S.op("dve", lambda e: e.memset(z[:], 0.0), writes=[z])
                S.dma("sp", lambda e: e.dma_start(out=xpad2[0:128, :], in_=z[:]), reads=[z])
                S.dma("sp", lambda e: e.dma_start(out=xpad2[SEQ + 128:SEQ + 256, :], in_=z[:]), reads=[z])
                S.barrier()
        for l in range(depth):
            xin = xpad if l == 0 else xpad2
            last = (l == depth - 1)
            for r in range(4):
                io = dict(cst)
                io.update({"xe": xin[r * TOK:r * TOK + TOK + 256, :], "w_in": w_in[l], "g": g_mix[l],
                           "edges": edges3[0 if r == 0 else (2 if r == 3 else 1)], "sinkb": sinkb[l],
                           "w_ap": w_ap[l], "fT": fTs[r], "maT": maTs[r], "sgbT": sgbTs[r]})
                build_A(ctx, io)
            for g in range(4):
                io = dict(cst)
                io.update({"fin_parts": [fTs[q, g].rearrange("c (t n) -> t c n", n=128) for q in range(4)],
                           "YT": YTs[g]})
                build_B(ctx, io)
            for r in range(4):
                io = dict(cst)
                io.update({"YTi": YTs[:, :, r * TOK:(r + 1) * TOK], "maT": maTs[r], "sgbT": sgbTs[r],
                           "x": xin[128 + r * TOK:128 + (r + 1) * TOK, :], "w_fp": w_fp[l], "w_out": w_out[l],
                           "gb": gffn[l], "w_rt": w_rt[l], "x1": x1s[r * TOK:(r + 1) * TOK, :],
                           "h2": h2s[r * TOK:(r + 1) * TOK, :], "aff": affs[r * TOK:(r + 1) * TOK, :],
                           "affT": affTs[:, r * TOK:(r + 1) * TOK]})
                build_C(ctx, io)
            for eg in range(4):
                io = dict(cst)
                io.update({"affT": affTs[4 * eg:4 * eg + 4, :], "h2": [h2s], "wg": wg[l, 4 * eg:4 * eg + 4],
                           "wu": wu[l, 4 * eg:4 * eg + 4], "wd": wd[l, 4 * eg:4 * eg + 4],
                           "y": [ys[4 * eg + p] for p in range(4)], "lst": lsts, "inv": invs,
                           "invT": invTs[4 * eg:4 * eg + 4], "thr": thr, "wcache": wcache})
                build_E(4, 1, ctx, io)
            for r in range(4):
                io = dict(cst)
                io.update({"x1": x1s[r * TOK:(r + 1) * TOK, :], "invT": invTs[:, :, r * 32:(r + 1) * 32],
                           "ys": ys, "gb": gfin,
                           "out": (out[r * TOK:(r + 1) * TOK, :] if last
                                   else xpad2[128 + r * TOK:128 + (r + 1) * TOK, :])})
                build_F(last, ctx, io)
        S.finish_all("sp")
        print("build_fused: %d instructions" % S.ninst, S.cnt)
    return nc


def host_inputs_fused(inp, depth=DEPTH):
    bucket, rel = _bucket_table()
    i = np.arange(128)
    edges3 = np.zeros((3, 128, 2, 128), np.float32)
    edges3[0, :, 0, :] = NEG
    edges3[2, :, 1, :] = NEG
    common = dict(host_consts_B())
    common.update({
        "biasg": np.ascontiguousarray(inp["rel_bias"][bucket].transpose(0, 3, 1, 2)).astype(np.float32),
        "maskw": np.where(np.abs(rel) <= 128, 0.0, NEG).astype(np.float32),
        "ident": np.eye(128, dtype=np.float32),
        "identf": np.eye(128, dtype=np.float32),
        "ustrict": (i[:, None] < i[None, :]).astype(np.float32),
        "bones": ((i[:, None] // 32) == (i[None, :] // 32)).astype(np.float32),
        "tokid": (i[:, None] * 128 + i[None, :]).astype(np.float32),
        "pidx": i[:, None].astype(np.float32),
        "edges3": edges3,
        "w_in": np.ascontiguousarray(inp["w_in"][:depth]),
        "g_mix": np.ascontiguousarray(inp["g_mix"][:depth].reshape(depth, 8, 128).transpose(0, 2, 1)),
        "sinkb": np.ascontiguousarray(np.broadcast_to(inp["attn_sink"][:depth, None, :], (depth, 128, 8))),
        "w_ap": np.ascontiguousarray(inp["w_attn_proj"][:depth]),
        "w_fp": np.ascontiguousarray(inp["w_fourier_proj"][:depth]),
        "w_out": np.ascontiguousarray(inp["w_out"][:depth]),
        "gffn": np.ascontiguousarray(np.broadcast_to(inp["g_ffn"][:depth, None, :], (depth, 128, D))),
        "w_rt": np.ascontiguousarray(inp["w_router"][:depth]),
        "wg": np.ascontiguousarray(inp["w_exp_gate"][:depth]),
        "wu": np.ascontiguousarray(inp["w_exp_up"][:depth]),
        "wd": np.ascontiguousarray(inp["w_exp_down"][:depth]),
        "gfin": np.ascontiguousarray(np.broadcast_to(inp["g_final"][None, :], (128, D))),
    })
    xp = []
    for b in range(NB):
        t = np.zeros((SEQ + 256, D), np.float32)
        t[128:128 + SEQ] = inp["x"][b]
        xp.append(t)
    maps = []
    for c in range(NCORES):
        m = dict(common)
        m["xpad"] = xp[c // 4]
        maps.append(m)
    return maps


def kernel_fused(depth=DEPTH, **inputs):
    inp = {k: np.asarray(v) for k, v in inputs.items()}
    res = _run(build_fused(depth), host_inputs_fused(inp, depth))
    return np.stack([np.asarray(res[0]["out"]), np.asarray(res[4]["out"])]).astype(np.float32)


def kernel(**inputs):
    return kernel_fused(DEPTH, **inputs)
```

```python
import numpy as np
import ml_dtypes
from contextlib import ExitStack
import concourse.bass as bass
import concourse.mybir as mybir
from concourse.bass_utils import run_bass_kernel_spmd

F32 = mybir.dt.float32
BF16 = mybir.dt.bfloat16
I32 = mybir.dt.int32
U32 = mybir.dt.uint32
ALU = mybir.AluOpType
AF = mybir.ActivationFunctionType
AX = mybir.AxisListType
NPBF = ml_dtypes.bfloat16

D = 1024
SEQ = 16384
NB = 2
DEPTH = 2
TOK = 4096
NCORES = 8
INW = 3328
DFF = 2816
NEXP = 16
CAP = 2048
EPS = 1e-6
NEG = -30000.0
NDMA = 40


class Buf:
    __slots__ = ("t", "w", "r", "name", "ws")

    def __init__(self, t, name=""):
        self.t = t
        self.w = None
        self.r = {}
        self.name = name
        self.ws = []

    def __getitem__(self, idx):
        return self.t[idx]


class Sched:
    def __init__(self, nc, stack):
        self.nc = nc
        self.stack = stack
        self.eng = {"pe": nc.tensor, "act": nc.scalar, "dve": nc.vector,
                    "pool": nc.gpsimd, "sp": nc.sync}
        self.sem = {k: stack.enter_context(nc.semaphore("s_" + k)) for k in self.eng}
        self.cnt = {k: 0 for k in self.eng}
        self.seen = {k: {} for k in self.eng}
        self.dsem = [stack.enter_context(nc.semaphore("d%d" % i)) for i in range(NDMA)]
        self.dval = [0] * NDMA
        self.drr = 0
        self.ninst = 0
        self.prefix = ""

    def sb(self, shape, dt, name):
        t = self.stack.enter_context(self.nc.sbuf_tensor(self.prefix + name, list(shape), dt))
        return Buf(t, name)

    def ps(self, shape, dt, name):
        t = self.stack.enter_context(self.nc.psum_tensor(self.prefix + name, list(shape), dt))
        return Buf(t, name)

    def barrier(self):
        snap = dict(self.cnt)
        for e in self.eng:
            for e2 in self.eng:
                if e2 != e:
                    self._wait(e, e2, snap[e2])
            for i in range(NDMA):
                self._wait(e, ("dma", i), self.dval[i])

    def _semobj(self, key):
        if isinstance(key, str):
            return self.sem[key]
        return self.dsem[key[1]]

    def _wait(self, eng, key, val):
        if val <= 0:
            return
        if self.seen[eng].get(key, 0) >= val:
            return
        self.eng[eng].wait_ge(self._semobj(key), val)
        self.seen[eng][key] = val

    def _deps(self, eng, reads, writes):
        deps = {}

        def add(t):
            if t is None:
                return
            k, v = t
            if deps.get(k, 0) < v:
                deps[k] = v
        for b in reads:
            add(b.w)
            for t in b.ws:
                add(t)
        for b in writes:
            add(b.w)
            for t in b.ws:
                add(t)
            for k, v in b.r.items():
                add((k, v))
        for k, v in deps.items():
            if k == eng and eng == "pe":
                continue
            self._wait(eng, k, v)

    def _mark(self, t, reads, writes):
        k, v = t
        for b in reads:
            if b.r.get(k, 0) < v:
                b.r[k] = v
        for b in writes:
            b.w = t
            b.r = {}
            b.ws = []

    def op(self, eng, fn, reads=(), writes=(), inc=True):
        self._deps(eng, reads, writes)
        inst = fn(self.eng[eng])
        if inc:
            self.cnt[eng] += 1
            inst.then_inc(self.sem[eng], 1)
            self._mark((eng, self.cnt[eng]), reads, writes)
        else:
            self._mark((eng, self.cnt[eng] + 1), reads, writes)
        self.ninst += 1
        return inst

    def dma(self, q, fn, reads=(), writes=(), also_writes=()):
        i = self.drr
        self.drr = (self.drr + 1) % NDMA
        key = ("dma", i)
        self._wait(q, key, self.dval[i])
        self._deps(q, reads, writes)
        inst = fn(self.eng[q])
        self.dval[i] += 16
        inst.then_inc(self.dsem[i], 16)
        self._mark((key, self.dval[i]), reads, writes)
        for b in also_writes:
            b.ws.append((key, self.dval[i]))
        self.ninst += 1
        return inst

    def finish(self, bufs, eng="sp"):
        for b in bufs:
            if b.w is not None:
                self._wait(eng, b.w[0], b.w[1])

    def finish_all(self, eng="sp"):
        for i in range(NDMA):
            self._wait(eng, ("dma", i), self.dval[i])


class Rot:
    def __init__(self, S, n, shape, dt, name, psum=False):
        mk = S.ps if psum else S.sb
        self.bufs = [mk(shape, dt, "%s%d" % (name, i)) for i in range(n)]
        self.i = 0

    def next(self):
        b = self.bufs[self.i]
        self.i = (self.i + 1) % len(self.bufs)
        return b


class Ctx:
    nc = None
    S = None
    nphase = 0


def _D(nc, io, name, shape, dt, kind):
    if io is not None:
        return io[name]
    return nc.dram_tensor(name, list(shape), dt, kind=kind).ap()


def _S(ctx, nc, st):
    if ctx is None:
        return Sched(nc, st)
    ctx.S.stack = st
    ctx.nphase += 1
    ctx.S.prefix = "p%d_" % ctx.nphase
    return ctx.S


def _end(ctx, S):
    if ctx is None:
        S.finish_all("sp")
    else:
        S.barrier()


def emit_norm_T(S, x_rows_ap, xblk, xn, tp_ps, hT, hT_dep, col0, g_sb, gcols, ssq, rstd, junk):
    S.dma("sp", lambda e: e.dma_start(out=xblk[:], in_=x_rows_ap), writes=[xblk])
    S.op("act", lambda e: e.activation(out=junk[:], in_=xblk[:], func=AF.Square,
                                       accum_out=ssq[:, 0:1]), reads=[xblk], writes=[junk, ssq])
    S.op("dve", lambda e: e.tensor_scalar(out=rstd[:, 0:1], in0=ssq[:, 0:1], scalar1=1.0 / D,
                                          scalar2=EPS, op0=ALU.mult, op1=ALU.add),
         reads=[ssq], writes=[rstd])
    S.op("act", lambda e: e.activation(out=rstd[:, 0:1], in_=rstd[:, 0:1], func=AF.Sqrt),
         reads=[rstd], writes=[rstd])
    S.op("dve", lambda e: e.reciprocal(out=rstd[:, 0:1], in_=rstd[:, 0:1]), reads=[rstd], writes=[rstd])
    S.op("dve", lambda e: e.tensor_scalar(out=xn[:], in0=xblk[:], scalar1=rstd[:, 0:1],
                                          scalar2=None, op0=ALU.mult), reads=[xblk, rstd], writes=[xn])
    for c in range(8):
        S.op("pe", lambda e, c=c: e.transpose(out=tp_ps[:, c, :], in_=xn[:, c * 128:(c + 1) * 128],
                                              identity=gcols["ident"][:]),
             reads=[xn, gcols["ident"]], writes=[tp_ps])
    S.op("dve", lambda e: e.tensor_tensor(out=hT[:, :, col0:col0 + 128], in0=tp_ps[:],
                                          in1=g_sb[:].unsqueeze(2).to_broadcast([128, 8, 128]),
                                          op=ALU.mult),
         reads=[tp_ps, g_sb], writes=[hT_dep])


def build_A(ctx=None, io=None):
    nc = ctx.nc if ctx is not None else bass.Bass("TRN2", target_bir_lowering=False)
    xe = _D(nc, io, "xe", [TOK + 256, D], F32, "ExternalInput")
    w_in = _D(nc, io, "w_in", [D, INW], F32, "ExternalInput")
    g_in = _D(nc, io, "g", [128, 8], F32, "ExternalInput")
    biasg = _D(nc, io, "biasg", [128, 8, 3, 128], F32, "ExternalInput")
    maskw = _D(nc, io, "maskw", [128, 3, 128], F32, "ExternalInput")
    edges = _D(nc, io, "edges", [128, 2, 128], F32, "ExternalInput")
    sinkb = _D(nc, io, "sinkb", [128, 8], F32, "ExternalInput")
    w_ap = _D(nc, io, "w_ap", [512, D], F32, "ExternalInput")
    ident_in = _D(nc, io, "ident", [128, 128], F32, "ExternalInput")
    fT = _D(nc, io, "fT", [4, 128, TOK], BF16, "ExternalOutput")
    maT = _D(nc, io, "maT", [8, 128, TOK], F32, "ExternalOutput")
    sgbT = _D(nc, io, "sgbT", [8, 128, TOK], F32, "ExternalOutput")
    outd = Buf(None, "outs")

    with ExitStack() as st:
        S = _S(ctx, nc, st)
        wkv = S.sb([128, 8, 256], BF16, "wkv")
        wr = S.sb([128, 8, 3072], BF16, "wr")
        wap = S.sb([128, 4, D], BF16, "wap")
        g_sb = S.sb([128, 8], F32, "g_sb")
        ident = S.sb([128, 128], BF16, "identb")
        kT = S.sb([128, TOK + 256], BF16, "kT")
        vaug = S.sb([128, 34, 2, 65], BF16, "vaug")
        bm = S.sb([128, 8, 3, 128], BF16, "bm")
        bfst = S.sb([128, 8, 128], BF16, "bfst")
        blst = S.sb([128, 8, 128], BF16, "blst")
        esink = S.sb([128, 8], F32, "esink")
        tmpb = S.sb([128, 8, 3, 128], F32, "tmpb")
        tmpm = S.sb([128, 3, 128], F32, "tmpm")
        tmpe = S.sb([128, 2, 128], F32, "tmpe")
        gc = {"ident": ident}

        S.dma("pool", lambda e: e.dma_start(out=ident[:], in_=ident_in), writes=[ident])
        S.dma("sp", lambda e: e.dma_start(out=g_sb[:], in_=g_in), writes=[g_sb])
        S.dma("sp", lambda e: e.dma_start(out=esink[:], in_=sinkb), writes=[esink])
        S.dma("sp", lambda e: e.dma_start(out=tmpb[:], in_=biasg), writes=[tmpb])
        S.dma("sp", lambda e: e.dma_start(out=tmpm[:], in_=maskw), writes=[tmpm])
        S.dma("sp", lambda e: e.dma_start(out=tmpe[:], in_=edges), writes=[tmpe])
        w_in_v = w_in.rearrange("(c p) e -> p c e", p=128)
        S.dma("pool", lambda e: e.dma_start(out=wkv[:], in_=w_in_v[:, :, 512:768]), writes=[wkv])
        for j in range(4):
            S.dma("pool", lambda e, j=j: e.dma_start(out=wr[:, :, j * 128:j * 128 + 64],
                                                     in_=w_in_v[:, :, j * 64:j * 64 + 64]), writes=[wr])
            S.dma("pool", lambda e, j=j: e.dma_start(out=wr[:, :, j * 128 + 64:j * 128 + 128],
                                                     in_=w_in_v[:, :, (4 + j) * 64:(4 + j) * 64 + 64]),
                  writes=[wr])
        for c in range(8):
            for (a, b) in ((768, 2048), (2048, 3328)):
                S.dma("pool", lambda e, c=c, a=a, b=b: e.dma_start(out=wr[:, c, a - 256:b - 256],
                                                                   in_=w_in_v[:, c, a:b]), writes=[wr])
        S.dma("pool", lambda e: e.dma_start(out=wap[:], in_=w_ap.rearrange("(c p) d -> p c d", p=128)),
              writes=[wap])
        S.op("dve", lambda e: e.tensor_tensor(out=tmpb[:], in0=tmpb[:],
                                              in1=tmpm[:].unsqueeze(1).to_broadcast([128, 8, 3, 128]),
                                              op=ALU.add), reads=[tmpb, tmpm], writes=[tmpb])
        S.op("dve", lambda e: e.tensor_copy(out=bm[:], in_=tmpb[:]), reads=[tmpb], writes=[bm])
        S.op("dve", lambda e: e.tensor_tensor(out=bfst[:], in0=tmpb[:, :, 0, :],
                                              in1=tmpe[:, 0:1, :].to_broadcast([128, 8, 128]),
                                              op=ALU.add), reads=[tmpb, tmpe], writes=[bfst])
        S.op("dve", lambda e: e.tensor_tensor(out=blst[:], in0=tmpb[:, :, 2, :],
                                              in1=tmpe[:, 1:2, :].to_broadcast([128, 8, 128]),
                                              op=ALU.add), reads=[tmpb, tmpe], writes=[blst])
        S.op("act", lambda e: e.activation(out=esink[:], in_=esink[:], func=AF.Exp),
             reads=[esink], writes=[esink])
        S.op("pool", lambda e: e.memset(vaug[:, :, :, 64:65], 1.0), writes=[vaug])

        xblk_r = Rot(S, 2, [128, D], F32, "xblk")
        xn_r = Rot(S, 2, [128, D], BF16, "xn")
        junk = S.sb([128, D], BF16, "junk")
        ssq_r = Rot(S, 2, [128, 1], F32, "ssq")
        rstd_r = Rot(S, 2, [128, 1], F32, "rstd")
        hT_r = Rot(S, 2, [128, 8, 512], BF16, "hT")
        tp_r = Rot(S, 1, [128, 8, 128], BF16, "tp", psum=True)
        zp_r = Rot(S, 2, [128, 512], F32, "zp", psum=True)
        st_r = Rot(S, 2, [128, 4, 128], F32, "stp", psum=True)
        pv_r = Rot(S, 2, [128, 512], F32, "pvp", psum=True)
        qT = S.sb([128, 4, 512], BF16, "qT")
        fo_r = Rot(S, 3, [128, 512], BF16, "fo")
        sg_r = Rot(S, 3, [128, 512], F32, "sg")
        ma_r = Rot(S, 3, [128, 512], F32, "ma")
        attnT = S.sb([128, 4, 512], BF16, "attnT")
        pT_r = Rot(S, 3, [128, 3, 128], BF16, "pT")
        attn_r = Rot(S, 2, [128, 8, 64], BF16, "attn")
        den_r = Rot(S, 2, [128, 8], F32, "den")

        def norm_chunk(e0, nblk):
            hT = hT_r.next()
            for b in range(nblk):
                emit_norm_T(S, xe[e0 + b * 128:e0 + (b + 1) * 128, :], xblk_r.next(), xn_r.next(),
                            tp_r.next(), hT, hT, b * 128, g_sb, gc, ssq_r.next(), rstd_r.next(), junk)
            return hT

        for ci in range(9):
            nblk = 4 if ci < 8 else 2
            n = nblk * 128
            e0 = ci * 512
            hT = norm_chunk(e0, nblk)
            zp = zp_r.next()
            for c in range(8):
                S.op("pe", lambda e, c=c: e.matmul(zp[:, 0:n], lhsT=wkv[:, c, 0:128], rhs=hT[:, c, 0:n],
                                                   start=(c == 0), stop=(c == 7)),
                     reads=[wkv, hT], writes=[zp])
            S.op("dve", lambda e: e.tensor_copy(out=kT[:, e0:e0 + n], in_=zp[:, 0:n]),
                 reads=[zp], writes=[kT])
            for b in range(nblk):
                zp = zp_r.next()
                blk = ci * 4 + b
                for c in range(8):
                    S.op("pe", lambda e, c=c, b=b: e.matmul(zp[:, 0:128], lhsT=hT[:, c, b * 128:(b + 1) * 128],
                                                            rhs=wkv[:, c, 128:256],
                                                            start=(c == 0), stop=(c == 7)),
                         reads=[wkv, hT], writes=[zp])
                S.op("act", lambda e, blk=blk: e.activation(
                    out=vaug[:, blk, :, 0:64], in_=zp[:, 0:128].rearrange("p (k d) -> p k d", k=2),
                    func=AF.Copy), reads=[zp], writes=[vaug])

        for s in range(8):
            t0 = s * 512
            hT = norm_chunk(128 + t0, 4)
            for j in range(16):
                zp = zp_r.next()
                col = j * 128 if j < 8 else 2048 + (j - 8) * 128
                for c in range(8):
                    S.op("pe", lambda e, c=c, col=col: e.matmul(zp[:], lhsT=wr[:, c, col:col + 128],
                                                                rhs=hT[:, c, :], start=(c == 0), stop=(c == 7)),
                         reads=[wr, hT], writes=[zp])
                if j < 4:
                    S.op("dve", lambda e, j=j: e.tensor_scalar(out=qT[:, j, :], in0=zp[:], scalar1=0.125,
                                                               scalar2=None, op0=ALU.mult),
                         reads=[zp], writes=[qT])
                elif j < 8:
                    fo = fo_r.next()
                    S.op("dve", lambda e: e.tensor_copy(out=fo[:], in_=zp[:]), reads=[zp], writes=[fo])
                    S.dma("pool", lambda e, j=j: e.dma_start(out=fT[j - 4, :, t0:t0 + 512], in_=fo[:]),
                          reads=[fo])
                else:
                    sg = sg_r.next()
                    S.op("act", lambda e: e.activation(out=sg[:], in_=zp[:], func=AF.Sigmoid),
                         reads=[zp], writes=[sg])
                    S.dma("pool", lambda e, j=j: e.dma_start(out=sgbT[j - 8, :, t0:t0 + 512], in_=sg[:]),
                          reads=[sg])
            for bq in range(4):
                n = s * 4 + bq
                pv = [pv_r.next(), pv_r.next()]
                pvv = [p[:, 0:260].rearrange("p (j d) -> p j d", d=65) for p in pv]
                for half in range(2):
                    p0 = 64 * half
                    for j in range(4):
                        h = j + 4 * half
                        stp = st_r.next()
                        for kb in range(3):
                            if n == 0 and kb == 0:
                                bias_ap = bfst[:, h, :]
                                bdep = bfst
                            elif n == 31 and kb == 2:
                                bias_ap = blst[:, h, :]
                                bdep = blst
                            else:
                                bias_ap = bm[:, h, kb, :]
                                bdep = bm
                            ke = (n + kb) * 128
                            S.op("pe", lambda e, kb=kb, ke=ke, j=j: e.matmul(
                                stp[:, kb, :], lhsT=kT[p0:p0 + 64, ke:ke + 128],
                                rhs=qT[p0:p0 + 64, j, bq * 128:(bq + 1) * 128], start=True, stop=False),
                                reads=[kT, qT], writes=[stp])
                            S.op("pe", lambda e, kb=kb, bias_ap=bias_ap: e.matmul(
                                stp[:, kb, :], lhsT=ident[:], rhs=bias_ap, start=False, stop=True),
                                reads=[ident, bdep], writes=[stp])
                        pT = pT_r.next()
                        S.op("act", lambda e: e.activation(out=pT[:], in_=stp[:, 0:3, :], func=AF.Exp),
                             reads=[stp], writes=[pT])
                        for kb in range(3):
                            S.op("pe", lambda e, kb=kb, j=j: e.matmul(
                                pvv[half][:, j, :], lhsT=pT[:, kb, :], rhs=vaug[:, n + kb, half, :],
                                start=(kb == 0), stop=(kb == 2)), reads=[pT, vaug], writes=[pv[half]])
                den = den_r.next()
                attn = attn_r.next()
                for half in range(2):
                    S.op("dve", lambda e, half=half: e.tensor_tensor(
                        out=den[:, 4 * half:4 * half + 4], in0=pvv[half][:, :, 64],
                        in1=esink[:, 4 * half:4 * half + 4], op=ALU.add),
                        reads=[pv[half], esink], writes=[den])
                S.op("dve", lambda e: e.reciprocal(out=den[:], in_=den[:]), reads=[den], writes=[den])
                for half in range(2):
                    S.op("dve", lambda e, half=half: e.tensor_tensor(
                        out=attn[:, 4 * half:4 * half + 4, :], in0=pvv[half][:, :, 0:64],
                        in1=den[:, 4 * half:4 * half + 4].unsqueeze(2).to_broadcast([128, 4, 64]),
                        op=ALU.mult), reads=[pv[half], den], writes=[attn])
                tp = tp_r.next()
                for c in range(4):
                    S.op("pe", lambda e, c=c: e.transpose(
                        out=tp[:, c, :], in_=attn[:, 2 * c:2 * c + 2, :].rearrange("p a d -> p (a d)"),
                        identity=ident[:]), reads=[attn, ident], writes=[tp])
                S.op("act", lambda e: e.activation(out=attnT[:, :, bq * 128:(bq + 1) * 128],
                                                   in_=tp[:, 0:4, :], func=AF.Copy),
                     reads=[tp], writes=[attnT])
            for dc in range(8):
                zp = zp_r.next()
                for c in range(8):
                    S.op("pe", lambda e, c=c, dc=dc: e.matmul(
                        zp[:], lhsT=wr[:, c, 1024 + dc * 128:1024 + (dc + 1) * 128], rhs=hT[:, c, :],
                        start=(c == 0), stop=(c == 7)), reads=[wr, hT], writes=[zp])
                sg = sg_r.next()
                S.op("act", lambda e: e.activation(out=sg[:], in_=zp[:], func=AF.Sigmoid),
                     reads=[zp], writes=[sg])
                zp2 = zp_r.next()
                for c in range(4):
                    S.op("pe", lambda e, c=c, dc=dc: e.matmul(
                        zp2[:], lhsT=wap[:, c, dc * 128:(dc + 1) * 128], rhs=attnT[:, c, :],
                        start=(c == 0), stop=(c == 3)), reads=[wap, attnT], writes=[zp2])
                ma = ma_r.next()
                S.op("dve", lambda e: e.tensor_tensor(out=ma[:], in0=zp2[:], in1=sg[:], op=ALU.mult),
                     reads=[zp2, sg], writes=[ma])
                S.dma("pool", lambda e, dc=dc: e.dma_start(out=maT[dc, :, t0:t0 + 512], in_=ma[:]),
                      reads=[ma])
        _end(ctx, S)
        print("build_A: %d instructions" % S.ninst, S.cnt)
    return nc


_CACHE = {}


def _bucket_table():
    if "bucket" in _CACHE:
        return _CACHE["bucket"]
    import jax
    import jax.numpy as jnp
    with jax.default_device(jax.devices("cpu")[0]):
        key = jnp.arange(128)[:, None, None]
        kb = jnp.arange(3)[None, :, None]
        q = jnp.arange(128)[None, None, :]
        rel = (kb * 128 + key - 128) - q
        half = 16
        max_exact = 8
        ret = (rel > 0).astype(jnp.int32) * half
        n = jnp.abs(rel)
        nf = jnp.maximum(n, 1).astype(jnp.float32)
        large = max_exact + (jnp.log(nf / max_exact) / np.float32(np.log(128 / max_exact))
                             * (half - max_exact)).astype(jnp.int32)
        large = jnp.minimum(large, half - 1)
        bucket = np.asarray(ret + jnp.where(n < max_exact, n, large))
        rel = np.asarray(rel)
    _CACHE["bucket"] = (bucket, rel)
    return _CACHE["bucket"]


def host_inputs_A(x, l, inp):
    bucket, rel = _bucket_table()
    rel_bias = inp["rel_bias"]
    biasg = np.ascontiguousarray(rel_bias[bucket].transpose(0, 3, 1, 2))
    maskw = np.where(np.abs(rel) <= 128, 0.0, NEG).astype(np.float32)
    common = {
        "w_in": np.ascontiguousarray(inp["w_in"][l]),
        "g": np.ascontiguousarray(inp["g_mix"][l].reshape(8, 128).T),
        "biasg": biasg.astype(np.float32),
        "maskw": maskw,
        "sinkb": np.ascontiguousarray(np.broadcast_to(inp["attn_sink"][l][None, :], (128, 8))),
        "w_ap": np.ascontiguousarray(inp["w_attn_proj"][l]),
        "ident": np.eye(128, dtype=np.float32),
    }
    maps = []
    for c in range(NCORES):
        b, r = divmod(c, 4)
        xe = np.zeros((TOK + 256, D), np.float32)
        lo = r * TOK - 128
        hi = lo + TOK + 256
        slo, shi = max(lo, 0), min(hi, SEQ)
        xe[slo - lo:shi - lo] = x[b, slo:shi]
        edges = np.zeros((128, 2, 128), np.float32)
        if r == 0:
            edges[:, 0, :] = NEG
        if r == 3:
            edges[:, 1, :] = NEG
        m = dict(common)
        m["xe"] = xe
        m["edges"] = edges
        maps.append(m)
    return maps


def build_B(ctx=None, io=None):
    nc = ctx.nc if ctx is not None else bass.Bass("TRN2", target_bir_lowering=False)
    fin = None if (io is not None and "fin_parts" in io) else _D(nc, io, "fin", [128, 128, 128], BF16, "ExternalInput")
    cs_in = _D(nc, io, "cs", [128, 256], BF16, "ExternalInput")
    cc_in = _D(nc, io, "ccsc", [128, 256], BF16, "ExternalInput")
    xk_in = _D(nc, io, "xk", [128, 128, 2, 256], BF16, "ExternalInput")
    YT = _D(nc, io, "YT", [128, SEQ], BF16, "ExternalOutput")
    scale = float(1.0 / np.sqrt(SEQ * 128.0))
    with ExitStack() as st:
        S = _S(ctx, nc, st)
        f_sb = S.sb([128, 128, 128], BF16, "f_sb")
        a_sb = S.sb([128, 2, 128, 128], BF16, "a_sb")
        y_sb = S.sb([128, SEQ], BF16, "y_sb")
        cs = S.sb([128, 256], BF16, "cs_sb")
        cc = S.sb([128, 256], BF16, "cc_sb")
        xk_r = Rot(S, 3, [128, 8, 2, 256], BF16, "xk")
        gg_r = Rot(S, 2, [128, 2, 4, 128], BF16, "gg")
        p1_r = Rot(S, 2, [128, 2, 256], F32, "p1", psum=True)
        p3_r = Rot(S, 4, [128, 2, 256], F32, "p3", psum=True)
        p4_r = Rot(S, 2, [128, 512], F32, "p4", psum=True)
        for q in range(4):
            if fin is None:
                S.dma("sp", lambda e, q=q: e.dma_start(out=f_sb[q * 32:(q + 1) * 32, :, :],
                                                       in_=io["fin_parts"][q]), writes=[f_sb])
            else:
                S.dma("sp", lambda e, q=q: e.dma_start(out=f_sb[:, q * 32:(q + 1) * 32, :],
                                                       in_=fin[:, q * 32:(q + 1) * 32, :]), writes=[f_sb])
        S.dma("sp", lambda e: e.dma_start(out=cs[:], in_=cs_in), writes=[cs])
        S.dma("sp", lambda e: e.dma_start(out=cc[:], in_=cc_in), writes=[cc])
        for c2 in range(64):
            p1 = p1_r.next()
            for i in range(2):
                c = c2 * 2 + i
                S.op("pe", lambda e, c=c, i=i: e.matmul(p1[:, i, :], lhsT=f_sb[:, c, :], rhs=cs[:],
                                                        start=True, stop=True), reads=[f_sb, cs], writes=[p1])
            eng = "dve" if c2 % 2 == 0 else "act"
            src = p1[:].rearrange("p c (r k) -> p r k c", r=2)
            dst = a_sb[:, :, :, c2 * 2:c2 * 2 + 2]
            if eng == "dve":
                S.op("dve", lambda e: e.tensor_copy(out=dst, in_=src), reads=[p1], writes=[a_sb])
            else:
                S.op("act", lambda e: e.activation(out=dst, in_=src, func=AF.Copy), reads=[p1], writes=[a_sb])
        yv = y_sb[:].rearrange("p (k2 k1) -> p k1 k2", k1=128)
        for g8 in range(16):
            xk = xk_r.next()
            S.dma("sp", lambda e, g8=g8: e.dma_start(out=xk[:], in_=xk_in[:, g8 * 8:(g8 + 1) * 8, :, :]),
                  writes=[xk])
            for g4 in range(2):
                gg = gg_r.next()
                for pr in range(2):
                    p3 = p3_r.next()
                    for i in range(2):
                        kk = g4 * 4 + pr * 2 + i
                        k1 = g8 * 8 + kk
                        S.op("pe", lambda e, k1=k1, kk=kk, i=i: e.matmul(
                            p3[:, i, :], lhsT=a_sb[:, 0, k1, :], rhs=xk[:, kk, 0, :], start=True, stop=False),
                            reads=[a_sb, xk], writes=[p3])
                        S.op("pe", lambda e, k1=k1, kk=kk, i=i: e.matmul(
                            p3[:, i, :], lhsT=a_sb[:, 1, k1, :], rhs=xk[:, kk, 1, :], start=False, stop=True),
                            reads=[a_sb, xk], writes=[p3])
                    src = p3[:].rearrange("p k (r q) -> p r k q", r=2)
                    dst = gg[:, :, pr * 2:pr * 2 + 2, :]
                    if pr == 0:
                        S.op("dve", lambda e: e.tensor_copy(out=dst, in_=src), reads=[p3], writes=[gg])
                    else:
                        S.op("act", lambda e: e.activation(out=dst, in_=src, func=AF.Copy),
                             reads=[p3], writes=[gg])
                p4 = p4_r.next()
                S.op("pe", lambda e: e.matmul(p4[:], lhsT=cc[:, 0:128],
                                              rhs=gg[:, 0, :, :].rearrange("p k q -> p (k q)"),
                                              start=True, stop=False), reads=[cc, gg], writes=[p4])
                S.op("pe", lambda e: e.matmul(p4[:], lhsT=cc[:, 128:256],
                                              rhs=gg[:, 1, :, :].rearrange("p k q -> p (k q)"),
                                              start=False, stop=True), reads=[cc, gg], writes=[p4])
                k1g = g8 * 8 + g4 * 4
                S.op("dve", lambda e, k1g=k1g: e.tensor_scalar(
                    out=yv[:, k1g:k1g + 4, :], in0=p4[:].rearrange("p (k q) -> p k q", k=4),
                    scalar1=scale, scalar2=None, op0=ALU.mult), reads=[p4], writes=[y_sb])
        for q in range(4):
            S.dma("sp", lambda e, q=q: e.dma_start(out=YT[:, q * 4096:(q + 1) * 4096],
                                                   in_=y_sb[:, q * 4096:(q + 1) * 4096]), reads=[y_sb])
        _end(ctx, S)
        print("build_B: %d instructions" % S.ninst, S.cnt)
    return nc


def host_consts_B():
    if "B" in _CACHE:
        return _CACHE["B"]
    j = np.arange(128, dtype=np.float64)
    ang = 2.0 * np.pi * np.outer(j, j) / 128.0
    C, Sn = np.cos(ang), np.sin(ang)
    cs = np.concatenate([C, -Sn], axis=1).astype(NPBF)
    ccsc = np.concatenate([C, Sn], axis=1).astype(NPBF)
    n2 = np.arange(128, dtype=np.float64)[:, None, None]
    k1 = np.arange(128, dtype=np.float64)[None, :, None]
    k2 = np.arange(128, dtype=np.float64)[None, None, :]
    kk = (k1 + 128.0 * k2)
    ph = 2.0 * np.pi * ((n2 * kk) % SEQ) / SEQ
    Rc, Rs = np.cos(ph), np.sin(ph)
    xk = np.empty((128, 128, 2, 256), dtype=NPBF)
    xk[:, :, 0, 0:128] = Rc.astype(NPBF)
    xk[:, :, 0, 128:256] = (-Rs).astype(NPBF)
    xk[:, :, 1, 0:128] = Rs.astype(NPBF)
    xk[:, :, 1, 128:256] = Rc.astype(NPBF)
    _CACHE["B"] = {"cs": cs, "ccsc": ccsc, "xk": xk}
    return _CACHE["B"]


def host_inputs_B(fT_list):
    cst = host_consts_B()
    maps = []
    for c in range(NCORES):
        b, g = divmod(c, 4)
        f_bg = np.concatenate([np.asarray(fT_list[b * 4 + r])[g] for r in range(4)], axis=1)
        fin = np.ascontiguousarray(f_bg.reshape(128, 128, 128).transpose(1, 0, 2))
        m = dict(cst)
        m["fin"] = fin
        maps.append(m)
    return maps


def build_C(ctx=None, io=None):
    nc = ctx.nc if ctx is not None else bass.Bass("TRN2", target_bir_lowering=False)
    YTi = _D(nc, io, "YTi", [4, 128, TOK], BF16, "ExternalInput")
    maT = _D(nc, io, "maT", [8, 128, TOK], F32, "ExternalInput")
    sgbT = _D(nc, io, "sgbT", [8, 128, TOK], F32, "ExternalInput")
    x = _D(nc, io, "x", [TOK, D], F32, "ExternalInput")
    w_fp = _D(nc, io, "w_fp", [512, D], F32, "ExternalInput")
    w_out = _D(nc, io, "w_out", [D, D], F32, "ExternalInput")
    gb_in = _D(nc, io, "gb", [128, D], F32, "ExternalInput")
    w_rt = _D(nc, io, "w_rt", [D, NEXP], F32, "ExternalInput")
    identf_in = _D(nc, io, "identf", [128, 128], F32, "ExternalInput")
    x1o = _D(nc, io, "x1", [TOK, D], F32, "ExternalOutput")
    h2o = _D(nc, io, "h2", [TOK, D], BF16, "ExternalOutput")
    affo = _D(nc, io, "aff", [TOK, NEXP], F32, "ExternalOutput")
    affTo = _D(nc, io, "affT", [NEXP, TOK], F32, "ExternalOutput")
    with ExitStack() as st:
        S = _S(ctx, nc, st)
        wfp = S.sb([128, 4, D], BF16, "wfp")
        wout = S.sb([128, 8, D], BF16, "wout")
        gb = S.sb([128, D], F32, "gbs")
        wrt = S.sb([128, 8, NEXP], F32, "wrt")
        identf = S.sb([128, 128], F32, "identf_sb")
        S.dma("pool", lambda e: e.dma_start(out=wfp[:], in_=w_fp.rearrange("(c p) d -> p c d", p=128)),
              writes=[wfp])
        for c in range(8):
            S.dma("pool", lambda e, c=c: e.dma_start(out=wout[:, c, :], in_=w_out[c * 128:(c + 1) * 128, :]),
                  writes=[wout])
        S.dma("sp", lambda e: e.dma_start(out=gb[:], in_=gb_in), writes=[gb])
        S.dma("sp", lambda e: e.dma_start(out=wrt[:], in_=w_rt.rearrange("(c p) e -> p c e", p=128)),
              writes=[wrt])
        S.dma("sp", lambda e: e.dma_start(out=identf[:], in_=identf_in), writes=[identf])
        yt_r = Rot(S, 2, [128, 4, 512], BF16, "yt")
        sg_r = Rot(S, 2, [128, 8, 512], F32, "sgt")
        ma_r = Rot(S, 2, [128, 8, 512], F32, "mat")
        mg_r = Rot(S, 2, [128, 8, 512], BF16, "mg")
        xb_r = Rot(S, 2, [128, D], F32, "xb")
        x1_r = Rot(S, 2, [128, D], F32, "x1t")
        h2f_r = Rot(S, 2, [128, D], F32, "h2f")
        h2b_r = Rot(S, 2, [128, D], BF16, "h2b")
        h2T_r = Rot(S, 2, [128, 8, 128], F32, "h2T")
        junk = S.sb([128, D], BF16, "junkc")
        ssq_r = Rot(S, 2, [128, 1], F32, "ssqc")
        rstd_r = Rot(S, 2, [128, 1], F32, "rstdc")
        mx_r = Rot(S, 2, [128, 1], F32, "mxc")
        sm_r = Rot(S, 2, [128, 1], F32, "smc")
        ex_r = Rot(S, 2, [128, NEXP], F32, "exc")
        af_r = Rot(S, 2, [128, NEXP], F32, "afc")
        aft_r = Rot(S, 2, [NEXP, 128], F32, "aftc")
        tmp_r = Rot(S, 2, [128, 512], F32, "tmpc")
        fo_p = Rot(S, 2, [128, 512], F32, "fop", psum=True)
        op_p = Rot(S, 2, [128, 512], F32, "opp", psum=True)
        tp_p = Rot(S, 2, [128, 4, 128], F32, "tpp", psum=True)
        rt_p = Rot(S, 1, [128, 512], F32, "rtp", psum=True)

        for s in range(8):
            t0 = s * 512
            yt, sg, ma, mg = yt_r.next(), sg_r.next(), ma_r.next(), mg_r.next()
            S.dma("sp", lambda e: e.dma_start(out=yt[:], in_=YTi[:, :, t0:t0 + 512].rearrange("c p t -> p c t")),
                  writes=[yt])
            S.dma("sp", lambda e: e.dma_start(out=sg[:], in_=sgbT[:, :, t0:t0 + 512].rearrange("c p t -> p c t")),
                  writes=[sg])
            S.dma("sp", lambda e: e.dma_start(out=ma[:], in_=maT[:, :, t0:t0 + 512].rearrange("c p t -> p c t")),
                  writes=[ma])
            for dc in range(8):
                fo = fo_p.next()
                for c in range(4):
                    S.op("pe", lambda e, c=c, dc=dc: e.matmul(fo[:], lhsT=wfp[:, c, dc * 128:(dc + 1) * 128],
                                                              rhs=yt[:, c, :], start=(c == 0), stop=(c == 3)),
                         reads=[wfp, yt], writes=[fo])
                tmp = tmp_r.next()
                S.op("dve", lambda e, dc=dc: e.tensor_tensor(out=tmp[:], in0=fo[:], in1=sg[:, dc, :], op=ALU.mult),
                     reads=[fo, sg], writes=[tmp])
                S.op("pool", lambda e, dc=dc: e.tensor_tensor(out=mg[:, dc, :], in0=tmp[:], in1=ma[:, dc, :],
                                                              op=ALU.add), reads=[tmp, ma], writes=[mg])
            for b in range(4):
                r0 = t0 + b * 128
                xb, x1t, h2f, h2b = xb_r.next(), x1_r.next(), h2f_r.next(), h2b_r.next()
                S.dma("sp", lambda e: e.dma_start(out=xb[:], in_=x[r0:r0 + 128, :]), writes=[xb])
                for hf in range(2):
                    op = op_p.next()
                    for c in range(8):
                        S.op("pe", lambda e, c=c, hf=hf: e.matmul(
                            op[:], lhsT=mg[:, c, b * 128:(b + 1) * 128], rhs=wout[:, c, hf * 512:(hf + 1) * 512],
                            start=(c == 0), stop=(c == 7)), reads=[mg, wout], writes=[op])
                    S.op("dve", lambda e, hf=hf: e.tensor_tensor(out=x1t[:, hf * 512:(hf + 1) * 512], in0=op[:],
                                                                 in1=xb[:, hf * 512:(hf + 1) * 512], op=ALU.add),
                         reads=[op, xb], writes=[x1t])
                S.dma("pool", lambda e: e.dma_start(out=x1o[r0:r0 + 128, :], in_=x1t[:]), reads=[x1t])
                ssq, rstd = ssq_r.next(), rstd_r.next()
                S.op("act", lambda e: e.activation(out=junk[:], in_=x1t[:], func=AF.Square, accum_out=ssq[:, 0:1]),
                     reads=[x1t], writes=[junk, ssq])
                S.op("dve", lambda e: e.tensor_scalar(out=rstd[:, 0:1], in0=ssq[:, 0:1], scalar1=1.0 / D,
                                                      scalar2=EPS, op0=ALU.mult, op1=ALU.add),
                     reads=[ssq], writes=[rstd])
                S.op("act", lambda e: e.activation(out=rstd[:, 0:1], in_=rstd[:, 0:1], func=AF.Sqrt),
                     reads=[rstd], writes=[rstd])
                S.op("dve", lambda e: e.reciprocal(out=rstd[:, 0:1], in_=rstd[:, 0:1]), reads=[rstd], writes=[rstd])
                S.op("dve", lambda e: e.scalar_tensor_tensor(out=h2f[:], in0=x1t[:], scalar=rstd[:, 0:1], in1=gb[:],
                                                             op0=ALU.mult, op1=ALU.mult),
                     reads=[x1t, rstd, gb], writes=[h2f])
                S.op("act", lambda e: e.activation(out=h2b[:], in_=h2f[:], func=AF.Copy), reads=[h2f], writes=[h2b])
                S.dma("pool", lambda e: e.dma_start(out=h2o[r0:r0 + 128, :], in_=h2b[:]), reads=[h2b])
                h2T = h2T_r.next()
                for q4 in range(2):
                    tp = tp_p.next()
                    for i in range(4):
                        c = q4 * 4 + i
                        S.op("pe", lambda e, c=c, i=i: e.transpose(out=tp[:, i, :], in_=h2f[:, c * 128:(c + 1) * 128],
                                                                   identity=identf[:]),
                             reads=[h2f, identf], writes=[tp])
                    S.op("dve", lambda e, q4=q4: e.tensor_copy(out=h2T[:, q4 * 4:(q4 + 1) * 4, :], in_=tp[:]),
                         reads=[tp], writes=[h2T])
                rt = rt_p.next()
                for c in range(8):
                    S.op("pe", lambda e, c=c: e.matmul(rt[:, 0:NEXP], lhsT=h2T[:, c, :], rhs=wrt[:, c, :],
                                                       start=(c == 0), stop=(c == 7)), reads=[h2T, wrt], writes=[rt])
                mx, sm, ex, af = mx_r.next(), sm_r.next(), ex_r.next(), af_r.next()
                S.op("dve", lambda e: e.reduce_max(out=mx[:, 0:1], in_=rt[:, 0:NEXP], axis=AX.X, negate=True),
                     reads=[rt], writes=[mx])
                S.op("act", lambda e: e.activation(out=ex[:], in_=rt[:, 0:NEXP], func=AF.Exp, bias=mx[:, 0:1],
                                                   accum_out=sm[:, 0:1]), reads=[rt, mx], writes=[ex, sm])
                S.op("dve", lambda e: e.reciprocal(out=sm[:, 0:1], in_=sm[:, 0:1]), reads=[sm], writes=[sm])
                S.op("dve", lambda e: e.tensor_scalar(out=af[:], in0=ex[:], scalar1=sm[:, 0:1], scalar2=None,
                                                      op0=ALU.mult), reads=[ex, sm], writes=[af])
                S.dma("pool", lambda e: e.dma_start(out=affo[r0:r0 + 128, :], in_=af[:]), reads=[af])
                tpa = tp_p.next()
                aft = aft_r.next()
                S.op("pe", lambda e: e.transpose(out=tpa[0:NEXP, 0, :], in_=af[:], identity=identf[:]),
                     reads=[af, identf], writes=[tpa])
                S.op("dve", lambda e: e.tensor_copy(out=aft[:], in_=tpa[0:NEXP, 0, :]), reads=[tpa], writes=[aft])
                S.dma("pool", lambda e: e.dma_start(out=affTo[:, r0:r0 + 128], in_=aft[:]), reads=[aft])
        _end(ctx, S)
        print("build_C: %d instructions" % S.ninst, S.cnt)
    return nc


def host_inputs_C(x, l, inp, YT_list, maT_list, sgbT_list):
    common = {
        "w_fp": np.ascontiguousarray(inp["w_fourier_proj"][l]),
        "w_out": np.ascontiguousarray(inp["w_out"][l]),
        "gb": np.ascontiguousarray(np.broadcast_to(inp["g_ffn"][l][None, :], (128, D))),
        "w_rt": np.ascontiguousarray(inp["w_router"][l]),
        "identf": np.eye(128, dtype=np.float32),
    }
    maps = []
    for c in range(NCORES):
        b, r = divmod(c, 4)
        yti = np.stack([np.asarray(YT_list[b * 4 + g])[:, r * TOK:(r + 1) * TOK] for g in range(4)], axis=0)
        m = dict(common)
        m["YTi"] = np.ascontiguousarray(yti)
        m["maT"] = np.asarray(maT_list[c])
        m["sgbT"] = np.asarray(sgbT_list[c])
        m["x"] = np.ascontiguousarray(x[b, r * TOK:(r + 1) * TOK])
        maps.append(m)
    return maps


NBIS = 36
FG = [(0, 4), (4, 4), (8, 4), (12, 4), (16, 4), (20, 2)]


def build_E(n_exp=2, n_b=NB, ctx=None, io=None):
    nc = ctx.nc if ctx is not None else bass.Bass("TRN2", target_bir_lowering=False)
    affT = _D(nc, io, "affT", [4, SEQ], F32, "ExternalInput")
    assert n_exp * n_b == 4
    h2 = io["h2"] if io is not None else [
        nc.dram_tensor("h2_%d" % b, [SEQ, D], BF16, kind="ExternalInput").ap() for b in range(n_b)]
    wg = _D(nc, io, "wg", [n_exp, D, DFF], F32, "ExternalInput")
    wu = _D(nc, io, "wu", [n_exp, D, DFF], F32, "ExternalInput")
    wd = _D(nc, io, "wd", [n_exp, DFF, D], F32, "ExternalInput")
    identf_in = _D(nc, io, "identf", [128, 128], F32, "ExternalInput")
    ustrict_in = _D(nc, io, "ustrict", [128, 128], F32, "ExternalInput")
    bones_in = _D(nc, io, "bones", [128, 128], F32, "ExternalInput")
    tokid_in = _D(nc, io, "tokid", [128, 128], F32, "ExternalInput")
    ident_in = _D(nc, io, "ident", [128, 128], F32, "ExternalInput")
    if io is not None:
        yo = io["y"]
        lst = io["lst"]
    else:
        yfull = nc.dram_tensor("y", [4, CAP + 1, D], BF16, kind="ExternalOutput").ap()
        yo = [yfull[p] for p in range(4)]
        lst = [nc.dram_tensor("lst%d" % p, [CAP + 128, 2], F32, kind="ExternalOutput").ap() for p in range(4)]
    invo = _D(nc, io, "inv", [4, SEQ], I32, "ExternalOutput")
    invTo = _D(nc, io, "invT", [4, 128, 128], I32, "ExternalOutput")
    pidx_in = _D(nc, io, "pidx", [128, 1], F32, "ExternalInput")
    thr = _D(nc, io, "thr", [128, 1], F32, "ExternalOutput")
    with ExitStack() as st:
        S = _S(ctx, nc, st)
        ustrict = S.sb([128, 128], F32, "ustrict_sb")
        bones = S.sb([128, 128], F32, "bones_sb")
        ident = S.sb([128, 128], BF16, "ident_sb")
        ones = S.sb([128, 128], F32, "ones_sb")
        identf = S.sb([128, 128], F32, "identf_sb")
        S.dma("sp", lambda e: e.dma_start(out=identf[:], in_=identf_in), writes=[identf])
        zrow = S.sb([1, D], BF16, "zrow")
        abis = S.sb([128, 512], F32, "abis")
        junk = S.sb([128, 512], F32, "junke")
        lo = S.sb([128, 1], F32, "lo")
        hi = S.sb([128, 1], F32, "hi")
        mid = S.sb([128, 1], F32, "mid")
        cp = S.sb([128, 1], F32, "cp")
        ge = S.sb([128, 1], F32, "ge")
        t2 = S.sb([128, 1], F32, "t2")
        thr_all = S.sb([128, 4], F32, "thr_all")
        cnt_p = Rot(S, 1, [128, 512], F32, "cntp", psum=True)
        thr_buf = Buf(None, "thr_d")
        lst_buf = [Buf(None, "lst%d" % p) for p in range(4)]

        S.dma("sp", lambda e: e.dma_start(out=ustrict[:], in_=ustrict_in), writes=[ustrict])
        S.dma("sp", lambda e: e.dma_start(out=bones[:], in_=bones_in), writes=[bones])
        S.dma("pool", lambda e: e.dma_start(out=ident[:], in_=ident_in), writes=[ident])
        S.dma("sp", lambda e: e.dma_start(out=abis[:], in_=affT.rearrange("p (r c) -> (p r) c", c=512)),
              writes=[abis])
        S.op("dve", lambda e: e.memset(ones[:], 1.0), writes=[ones])
        S.op("dve", lambda e: e.memset(zrow[:], 0.0), writes=[zrow])
        S.op("dve", lambda e: e.memset(lo[:], 0.0), writes=[lo])
        S.op("dve", lambda e: e.memset(hi[:], 1.0), writes=[hi])
        for p in range(4):
            S.dma("sp", lambda e, p=p: e.dma_start(out=yo[p][CAP:CAP + 1, :], in_=zrow[:]), reads=[zrow])
        for it in range(NBIS):
            cps = cnt_p.next()
            S.op("dve", lambda e: e.tensor_tensor(out=mid[:], in0=lo[:], in1=hi[:], op=ALU.add),
                 reads=[lo, hi], writes=[mid])
            S.op("dve", lambda e: e.tensor_scalar(out=mid[:], in0=mid[:], scalar1=0.5, scalar2=None, op0=ALU.mult),
                 reads=[mid], writes=[mid])
            S.op("dve", lambda e: e.tensor_scalar(out=junk[:], in0=abis[:], scalar1=mid[:, 0:1], scalar2=0.0,
                                                  op0=ALU.is_ge, op1=ALU.add, accum_out=cp[:, 0:1]),
                 reads=[abis, mid], writes=[junk, cp])
            S.op("pe", lambda e: e.matmul(cps[:, 0:1], lhsT=bones[:], rhs=cp[:, 0:1], start=True, stop=True),
                 reads=[bones, cp], writes=[cps])
            S.op("dve", lambda e: e.tensor_scalar(out=ge[:], in0=cps[:, 0:1], scalar1=float(CAP) - 0.5, scalar2=None,
                                                  op0=ALU.is_ge), reads=[cps], writes=[ge])
            S.op("dve", lambda e: e.scalar_tensor_tensor(out=lo[:], in0=mid[:], scalar=ge[:, 0:1], in1=lo[:],
                                                         op0=ALU.mult, op1=ALU.max),
                 reads=[mid, ge, lo], writes=[lo])
            S.op("dve", lambda e: e.scalar_tensor_tensor(out=t2[:], in0=ge[:], scalar=2.0, in1=mid[:],
                                                         op0=ALU.mult, op1=ALU.add),
                 reads=[mid, ge], writes=[t2])
            S.op("dve", lambda e: e.tensor_tensor(out=hi[:], in0=hi[:], in1=t2[:], op=ALU.min),
                 reads=[hi, t2], writes=[hi])
        S.dma("sp", lambda e: e.dma_start(out=thr, in_=lo[:]), reads=[lo], writes=[thr_buf])
        S.dma("sp", lambda e: e.dma_start(
            out=thr_all[:], in_=thr.rearrange("(p r) o -> r (p o)", r=32)[0, :].partition_broadcast(128),
            allow_slow_non_contiguous=True),
            reads=[thr_buf], writes=[thr_all])

        atok = S.sb([128, 128], F32, "atok")
        mask = S.sb([128, 128], F32, "mask")
        csum = S.sb([128, 128], F32, "csum")
        posf = S.sb([128, 128], F32, "posf")
        offs = S.sb([128, 1], F32, "offs")
        pay = S.sb([128, 128, 2], F32, "pay")
        invi = S.sb([128, 128], I32, "invi")
        invTi = S.sb([128, 128], I32, "invTi")
        lsb2 = [S.sb([128, 16, 2], F32, "lsb%d" % k) for k in range(2)]
        idsi2 = [S.sb([128, 16], I32, "idsi%d" % k) for k in range(2)]
        gates2 = [S.sb([128, 16], F32, "gates%d" % k) for k in range(2)]
        xs4 = [S.sb([128, D], BF16, "xs%d" % k) for k in range(4)]
        xsT_r = Rot(S, 2, [128, 8, 512], BF16, "xsT")
        actT = S.sb([128, 22, 512], BF16, "actT")
        wd_r = Rot(S, 2, [128, 22, D], BF16, "wd_sb")
        wg_r = Rot(S, 2, [128, 8, 512], BF16, "wg_t")
        wu_r = Rot(S, 2, [128, 8, 512], BF16, "wu_t")
        sl_r = Rot(S, 2, [128, 512], F32, "sl")
        yt_r = Rot(S, 2, [128, D], BF16, "yt")
        tp_p = Rot(S, 1, [128, 8, 128], BF16, "tpe", psum=True)
        hg_p = Rot(S, 2, [128, 512], F32, "hgp", psum=True)
        hu_p = Rot(S, 2, [128, 512], F32, "hup", psum=True)
        yp_p = Rot(S, 2, [128, 512], F32, "ypp", psum=True)
        S.dma("sp", lambda e: e.dma_start(out=pay[:, :, 0], in_=tokid_in, allow_slow_non_contiguous=True), writes=[pay])
        pidx = S.sb([128, 1], F32, "pidx_sb")
        sidx = S.sb([128, 128], F32, "sidx")
        sidi = S.sb([128, 128], I32, "sidi")
        S.dma("sp", lambda e: e.dma_start(out=pidx[:], in_=pidx_in), writes=[pidx])
        problems = [(ei, b) for ei in range(n_exp) for b in range(n_b)]

        def s1_chunks(p):
            par = p % 2
            lsb, idsi, gates = lsb2[par], idsi2[par], gates2[par]

            def prep():
                S.dma("sp", lambda e: e.dma_start(out=atok[:], in_=affT[p].rearrange("(i j) -> i j", j=128)),
                      writes=[atok])
                S.op("dve", lambda e: e.tensor_scalar(out=mask[:], in0=atok[:], scalar1=thr_all[:, p:p + 1],
                                                      scalar2=None, op0=ALU.is_ge),
                     reads=[atok, thr_all], writes=[mask])
                S.op("dve", lambda e: e.tensor_tensor_scan(out=csum[:], data0=ones[:], data1=mask[:], initial=0.0,
                                                           op0=ALU.mult, op1=ALU.add),
                     reads=[ones, mask], writes=[csum])
                cps = cnt_p.next()
                S.op("pe", lambda e: e.matmul(cps[:, 0:1], lhsT=ustrict[:], rhs=csum[:, 127:128], start=True, stop=True),
                     reads=[ustrict, csum], writes=[cps])
                S.op("dve", lambda e: e.tensor_copy(out=offs[:], in_=cps[:, 0:1]), reads=[cps], writes=[offs])
                S.op("dve", lambda e: e.tensor_scalar(out=posf[:], in0=csum[:], scalar1=offs[:, 0:1],
                                                      scalar2=-1.0 - CAP, op0=ALU.add, op1=ALU.add),
                     reads=[csum, offs], writes=[posf])
                S.op("dve", lambda e: e.tensor_tensor(out=posf[:], in0=posf[:], in1=mask[:], op=ALU.mult),
                     reads=[posf, mask], writes=[posf])
                S.op("dve", lambda e: e.tensor_scalar(out=posf[:], in0=posf[:], scalar1=float(CAP), scalar2=float(CAP),
                                                      op0=ALU.add, op1=ALU.min), reads=[posf], writes=[posf])
                S.op("dve", lambda e: e.tensor_copy(out=invi[:], in_=posf[:]), reads=[posf], writes=[invi])
                S.op("dve", lambda e: e.tensor_scalar(out=sidx[:], in0=mask[:], scalar1=-1.0, scalar2=1.0,
                                                      op0=ALU.mult, op1=ALU.add), reads=[mask], writes=[sidx])
                S.op("dve", lambda e: e.scalar_tensor_tensor(out=sidx[:], in0=sidx[:], scalar=pidx[:, 0:1], in1=posf[:],
                                                             op0=ALU.mult, op1=ALU.add),
                     reads=[sidx, pidx, posf], writes=[sidx])
                S.op("dve", lambda e: e.tensor_copy(out=sidi[:], in_=sidx[:]), reads=[sidx], writes=[sidi])
                S.op("dve", lambda e: e.tensor_copy(out=pay[:, :, 1], in_=atok[:]), reads=[atok], writes=[pay])
                S.dma("sp", lambda e: e.dma_start(out=invo[p].rearrange("(i j) -> i j", j=128), in_=invi[:]),
                      reads=[invi])
                cpt = cnt_p.next()
                S.op("pe", lambda e: e.transpose(out=cpt[:, 0:128], in_=posf[:], identity=identf[:]),
                     reads=[posf, identf], writes=[cpt])
                S.op("dve", lambda e: e.tensor_copy(out=invTi[:], in_=cpt[:, 0:128]), reads=[cpt], writes=[invTi])
                S.dma("sp", lambda e: e.dma_start(out=invTo[p], in_=invTi[:]), reads=[invTi])

            def scat(k):
                for j in range(16 * k, 16 * k + 16):
                    S.dma("pool", lambda e, j=j: e.indirect_dma_start(
                        out=lst[p], out_offset=bass.IndirectOffsetOnAxis(ap=sidi[:, j:j + 1], axis=0),
                        in_=pay[:, j, :], in_offset=None), reads=[sidi, pay], also_writes=[lst_buf[p]])

            def fin():
                S.dma("sp", lambda e: e.dma_start(out=lsb[:], in_=lst[p][0:CAP, :].rearrange("(t s) two -> s t two", s=128)),
                      reads=[lst_buf[p]], writes=[lsb])
                S.op("dve", lambda e: e.tensor_copy(out=idsi[:], in_=lsb[:, :, 0]), reads=[lsb], writes=[idsi])
                S.op("dve", lambda e: e.tensor_copy(out=gates[:], in_=lsb[:, :, 1]), reads=[lsb], writes=[gates])
            return [prep] + [(lambda k=k: scat(k)) for k in range(8)] + [fin]

        def emit_gathers(pp, sgg):
            bb = problems[pp][1]
            ids_ = idsi2[pp % 2]
            for t4 in range(4):
                t = sgg * 4 + t4
                S.dma("pool", lambda e, t=t, t4=t4: e.indirect_dma_start(
                    out=xs4[t4][:], out_offset=None, in_=h2[bb],
                    in_offset=bass.IndirectOffsetOnAxis(ap=ids_[:, t:t + 1], axis=0)),
                    reads=[ids_], writes=[xs4[t4]])

        def wd_load(ei, wd_sb, lo, hi):
            for c in range(lo, hi):
                S.dma("pool", lambda e, c=c: e.dma_start(out=wd_sb[:, c, :], in_=wd[ei, c * 128:(c + 1) * 128, :]),
                      writes=[wd_sb])

        wd_cur = wd_r.next()
        wd_load(problems[0][0], wd_cur, 0, 22)
        for th in s1_chunks(0):
            th()
        for p, (ei, b) in enumerate(problems):
            par = p % 2
            idsi, gates = idsi2[par], gates2[par]
            inter = []
            if p + 1 < len(problems):
                inter = s1_chunks(p + 1)
                if problems[p + 1][0] != ei:
                    wd_nxt = wd_r.next()
                    fin_th = inter.pop()
                    for (lo_, hi_) in ((0, 6), (6, 12), (12, 17), (17, 22)):
                        inter.append(lambda wd_nxt=wd_nxt, ne=problems[p + 1][0], lo_=lo_, hi_=hi_: wd_load(ne, wd_nxt, lo_, hi_))
                    inter.append(fin_th)
                else:
                    wd_nxt = wd_cur
            slot = 0
            if p == 0:
                emit_gathers(0, 0)
            for sg in range(4):
                xsT = xsT_r.next()
                for t4 in range(4):
                    t = sg * 4 + t4
                    xs = xs4[t4]
                    tp = tp_p.next()
                    for c in range(8):
                        S.op("pe", lambda e, c=c: e.transpose(out=tp[:, c, :], in_=xs[:, c * 128:(c + 1) * 128],
                                                              identity=ident[:]), reads=[xs, ident], writes=[tp],
                             inc=(c == 7))
                    S.op("dve", lambda e, t4=t4: e.tensor_copy(out=xsT[:, :, t4 * 128:(t4 + 1) * 128], in_=tp[:]),
                         reads=[tp], writes=[xsT])
                for (c0, ncn) in FG:
                    wgt, wut = wg_r.next(), wu_r.next()
                    f0, fn = c0 * 128, ncn * 128
                    S.dma("pool", lambda e: e.dma_start(
                        out=wgt[:, :, 0:fn], in_=wg[ei, :, f0:f0 + fn].rearrange("(c q) f -> q c f", q=128)),
                        writes=[wgt])
                    S.dma("pool", lambda e: e.dma_start(
                        out=wut[:, :, 0:fn], in_=wu[ei, :, f0:f0 + fn].rearrange("(c q) f -> q c f", q=128)),
                        writes=[wut])
                    if slot % 2 == 1 and inter:
                        inter.pop(0)()
                    slot += 1
                    for fc in range(ncn):
                        hg, hu = hg_p.next(), hu_p.next()
                        for c in range(8):
                            S.op("pe", lambda e, c=c, fc=fc: e.matmul(
                                hg[:], lhsT=wgt[:, c, fc * 128:(fc + 1) * 128], rhs=xsT[:, c, :],
                                start=(c == 0), stop=(c == 7)), reads=[wgt, xsT], writes=[hg], inc=(c == 7))
                        for c in range(8):
                            S.op("pe", lambda e, c=c, fc=fc: e.matmul(
                                hu[:], lhsT=wut[:, c, fc * 128:(fc + 1) * 128], rhs=xsT[:, c, :],
                                start=(c == 0), stop=(c == 7)), reads=[wut, xsT], writes=[hu], inc=(c == 7))
                        sl = sl_r.next()
                        S.op("act", lambda e: e.activation(out=sl[:], in_=hg[:], func=AF.Silu),
                             reads=[hg], writes=[sl])
                        S.op("dve", lambda e, fc=fc, c0=c0: e.tensor_tensor(
                            out=actT[:, c0 + fc, :], in0=hu[:], in1=sl[:], op=ALU.mult),
                            reads=[hu, sl], writes=[actT])
                if sg < 3:
                    emit_gathers(p, sg + 1)
                elif p + 1 < len(problems):
                    while inter:
                        inter.pop(0)()
                    emit_gathers(p + 1, 0)
                for t4 in range(4):
                    t = sg * 4 + t4
                    yt = yt_r.next()
                    for hf in range(2):
                        yp = yp_p.next()
                        for fcc in range(22):
                            S.op("pe", lambda e, fcc=fcc, hf=hf, t4=t4: e.matmul(
                                yp[:], lhsT=actT[:, fcc, t4 * 128:(t4 + 1) * 128],
                                rhs=wd_cur[:, fcc, hf * 512:(hf + 1) * 512],
                                start=(fcc == 0), stop=(fcc == 21)), reads=[actT, wd_cur], writes=[yp],
                                inc=(fcc == 21))
                        S.op("act", lambda e, hf=hf, t=t: e.activation(
                            out=yt[:, hf * 512:(hf + 1) * 512], in_=yp[:], func=AF.Copy, scale=gates[:, t:t + 1]),
                            reads=[yp, gates], writes=[yt])
                    S.dma("sp", lambda e, t=t: e.dma_start(out=yo[p][t * 128:(t + 1) * 128, :], in_=yt[:]),
                          reads=[yt])
            while inter:
                inter.pop(0)()
            if p + 1 < len(problems):
                wd_cur = wd_nxt
        _end(ctx, S)
        print("build_E: %d instructions" % S.ninst, S.cnt)
    return nc


def host_inputs_E(l, inp, affT_list, h2_list):
    affT = np.stack([np.concatenate([np.asarray(affT_list[b * 4 + r]) for r in range(4)], axis=1) for b in range(NB)])
    h2 = np.stack([np.concatenate([np.asarray(h2_list[b * 4 + r]) for r in range(4)], axis=0) for b in range(NB)])
    i = np.arange(128)
    common = {
        "h2_0": h2[0], "h2_1": h2[1],
        "ustrict": (i[:, None] < i[None, :]).astype(np.float32),
        "bones": ((i[:, None] // 32) == (i[None, :] // 32)).astype(np.float32),
        "tokid": (i[:, None] * 128 + i[None, :]).astype(np.float32),
        "pidx": i[:, None].astype(np.float32),
        "ident": np.eye(128, dtype=np.float32),
        "identf": np.eye(128, dtype=np.float32),
    }
    maps = []
    for c in range(NCORES):
        m = dict(common)
        m["affT"] = np.ascontiguousarray(np.stack([affT[b, 2 * c + ei] for ei in range(2) for b in range(NB)]))
        m["wg"] = np.ascontiguousarray(inp["w_exp_gate"][l, 2 * c:2 * c + 2])
        m["wu"] = np.ascontiguousarray(inp["w_exp_up"][l, 2 * c:2 * c + 2])
        m["wd"] = np.ascontiguousarray(inp["w_exp_down"][l, 2 * c:2 * c + 2])
        maps.append(m)
    return maps


def build_F(final, ctx=None, io=None):
    nc = ctx.nc if ctx is not None else bass.Bass("TRN2", target_bir_lowering=False)
    x1 = _D(nc, io, "x1", [TOK, D], F32, "ExternalInput")
    invT = _D(nc, io, "invT", [NEXP, 128, 32], I32, "ExternalInput")
    ys = io["ys"] if io is not None else [
        nc.dram_tensor("y%d" % e, [CAP + 1, D], BF16, kind="ExternalInput").ap() for e in range(NEXP)]
    ident_in = _D(nc, io, "ident", [128, 128], F32, "ExternalInput")
    gb_in = _D(nc, io, "gb", [128, D], F32, "ExternalInput")
    out = _D(nc, io, "out", [TOK, D], F32, "ExternalOutput")
    with ExitStack() as st:
        S = _S(ctx, nc, st)
        ident = S.sb([128, 128], BF16, "ident_sb")
        gb = S.sb([128, D], F32, "gb_sb")
        inv_sb = S.sb([128, NEXP, 32], I32, "inv_sb")
        S.dma("pool", lambda e: e.dma_start(out=ident[:], in_=ident_in), writes=[ident])
        S.dma("sp", lambda e: e.dma_start(out=gb[:], in_=gb_in), writes=[gb])
        for ex in range(NEXP):
            S.dma("sp", lambda e, ex=ex: e.dma_start(out=inv_sb[:, ex, :], in_=invT[ex]), writes=[inv_sb])
        stg_r = Rot(S, 12, [128, D], BF16, "stg")
        xb_r = Rot(S, 2, [128, D], F32, "xbf")
        x2_r = Rot(S, 2, [128, D], F32, "x2f")
        o_r = Rot(S, 2, [128, D], F32, "of")
        junk = S.sb([128, D], BF16, "junkf")
        ssq_r = Rot(S, 2, [128, 1], F32, "ssqf")
        rstd_r = Rot(S, 2, [128, 1], F32, "rstdf")
        acc_p = Rot(S, 4, [128, 512], F32, "accp", psum=True)
        for t in range(32):
            r0 = t * 128
            xb, x2 = xb_r.next(), x2_r.next()
            S.dma("sp", lambda e: e.dma_start(out=xb[:], in_=x1[r0:r0 + 128, :]), writes=[xb])
            acc = [acc_p.next(), acc_p.next()]
            for ex in range(NEXP):
                stg = stg_r.next()
                S.dma("pool", lambda e, ex=ex: e.indirect_dma_start(
                    out=stg[:], out_offset=None, in_=ys[ex],
                    in_offset=bass.IndirectOffsetOnAxis(ap=inv_sb[:, ex, t:t + 1], axis=0)),
                    reads=[inv_sb], writes=[stg])
                for hf in range(2):
                    S.op("pe", lambda e, hf=hf, ex=ex: e.matmul(acc[hf][:], lhsT=ident[:],
                                                                rhs=stg[:, hf * 512:(hf + 1) * 512],
                                                                start=(ex == 0), stop=(ex == NEXP - 1)),
                         reads=[ident, stg], writes=[acc[hf]])
            for hf in range(2):
                S.op("dve", lambda e, hf=hf: e.tensor_tensor(out=x2[:, hf * 512:(hf + 1) * 512], in0=acc[hf][:],
                                                             in1=xb[:, hf * 512:(hf + 1) * 512], op=ALU.add),
                     reads=[acc[hf], xb], writes=[x2])
            if not final:
                S.dma("sp", lambda e: e.dma_start(out=out[r0:r0 + 128, :], in_=x2[:]), reads=[x2])
            else:
                ssq, rstd, o = ssq_r.next(), rstd_r.next(), o_r.next()
                S.op("act", lambda e: e.activation(out=junk[:], in_=x2[:], func=AF.Square, accum_out=ssq[:, 0:1]),
                     reads=[x2], writes=[junk, ssq])
                S.op("dve", lambda e: e.tensor_scalar(out=rstd[:, 0:1], in0=ssq[:, 0:1], scalar1=1.0 / D,
                                                      scalar2=EPS, op0=ALU.mult, op1=ALU.add),
                     reads=[ssq], writes=[rstd])
                S.op("act", lambda e: e.activation(out=rstd[:, 0:1], in_=rstd[:, 0:1], func=AF.Sqrt),
                     reads=[rstd], writes=[rstd])
                S.op("dve", lambda e: e.reciprocal(out=rstd[:, 0:1], in_=rstd[:, 0:1]), reads=[rstd], writes=[rstd])
                S.op("dve", lambda e: e.scalar_tensor_tensor(out=o[:], in0=x2[:], scalar=rstd[:, 0:1], in1=gb[:],
                                                             op0=ALU.mult, op1=ALU.mult),
                     reads=[x2, rstd, gb], writes=[o])
                S.dma("sp", lambda e: e.dma_start(out=out[r0:r0 + 128, :], in_=o[:]), reads=[o])
        _end(ctx, S)
        print("build_F: %d instructions" % S.ninst, S.cnt)
    return nc


def host_inputs_F(inp, x1_list, y_list, invT_list):
    gb = np.ascontiguousarray(np.broadcast_to(inp["g_final"][None, :], (128, D)))
    ident = np.eye(128, dtype=np.float32)
    maps = []
    for c in range(NCORES):
        b, r = divmod(c, 4)
        m = {"x1": np.asarray(x1_list[c]), "ident": ident, "gb": gb}
        invT = np.empty((NEXP, 128, 32), np.int32)
        for ex in range(NEXP):
            cc, ei = divmod(ex, 2)
            p = ei * 2 + b
            m["y%d" % ex] = np.ascontiguousarray(np.asarray(y_list[cc])[p])
            invT[ex] = np.asarray(invT_list[cc])[p][:, r * 32:(r + 1) * 32]
        m["invT"] = invT
        maps.append(m)
    return maps


def _run(nc, maps):
    res = run_bass_kernel_spmd(nc, maps, core_ids=list(range(NCORES)))
    return res.results


def kernel_unfused(**inputs):
    inp = {k: np.asarray(v) for k, v in inputs.items()}
    x = np.ascontiguousarray(inp["x"], dtype=np.float32)
    for l in range(DEPTH):
        rA = _run(build_A(), host_inputs_A(x, l, inp))
        rB = _run(build_B(), host_inputs_B([r["fT"] for r in rA]))
        rC = _run(build_C(), host_inputs_C(x, l, inp, [r["YT"] for r in rB], [r["maT"] for r in rA],
                                           [r["sgbT"] for r in rA]))
        del rA, rB
        rE = _run(build_E(), host_inputs_E(l, inp, [r["affT"] for r in rC], [r["h2"] for r in rC]))
        rF = _run(build_F(l == DEPTH - 1), host_inputs_F(inp, [r["x1"] for r in rC], [r["y"] for r in rE],
                                                          [r["invT"] for r in rE]))
        del rC, rE
        x = np.stack([np.concatenate([np.asarray(rF[b * 4 + r]["out"]) for r in range(4)], axis=0)
                      for b in range(NB)])
    return x.astype(np.float32)


def build_fused(depth=DEPTH):
    nc = bass.Bass("TRN2", target_bir_lowering=False)
    ctx = Ctx()
    ctx.nc = nc
    EI, IN, EO = "ExternalInput", "Internal", "ExternalOutput"

    def dt_(name, shape, dt, kind):
        return nc.dram_tensor(name, list(shape), dt, kind=kind).ap()
    xpad = dt_("xpad", [SEQ + 256, D], F32, EI)
    cst = {
        "biasg": dt_("biasg", [128, 8, 3, 128], F32, EI),
        "maskw": dt_("maskw", [128, 3, 128], F32, EI),
        "ident": dt_("ident", [128, 128], F32, EI),
        "identf": dt_("identf", [128, 128], F32, EI),
        "cs": dt_("cs", [128, 256], BF16, EI),
        "ccsc": dt_("ccsc", [128, 256], BF16, EI),
        "xk": dt_("xk", [128, 128, 2, 256], BF16, EI),
        "ustrict": dt_("ustrict", [128, 128], F32, EI),
        "bones": dt_("bones", [128, 128], F32, EI),
        "tokid": dt_("tokid", [128, 128], F32, EI),
        "pidx": dt_("pidx", [128, 1], F32, EI),
    }
    edges3 = dt_("edges3", [3, 128, 2, 128], F32, EI)
    w_in = dt_("w_in", [depth, D, INW], F32, EI)
    g_mix = dt_("g_mix", [depth, 128, 8], F32, EI)
    sinkb = dt_("sinkb", [depth, 128, 8], F32, EI)
    w_ap = dt_("w_ap", [depth, 512, D], F32, EI)
    w_fp = dt_("w_fp", [depth, 512, D], F32, EI)
    w_out = dt_("w_out", [depth, D, D], F32, EI)
    gffn = dt_("gffn", [depth, 128, D], F32, EI)
    w_rt = dt_("w_rt", [depth, D, NEXP], F32, EI)
    wg = dt_("wg", [depth, NEXP, D, DFF], F32, EI)
    wu = dt_("wu", [depth, NEXP, D, DFF], F32, EI)
    wd = dt_("wd", [depth, NEXP, DFF, D], F32, EI)
    gfin = dt_("gfin", [128, D], F32, EI)
    out = dt_("out", [SEQ, D], F32, EO)
    fTs = dt_("s_fT", [4, 4, 128, TOK], BF16, IN)
    maTs = dt_("s_maT", [4, 8, 128, TOK], F32, IN)
    sgbTs = dt_("s_sgbT", [4, 8, 128, TOK], F32, IN)
    YTs = dt_("s_YT", [4, 128, SEQ], BF16, IN)
    x1s = dt_("s_x1", [SEQ, D], F32, IN)
    h2s = dt_("s_h2", [SEQ, D], BF16, IN)
    affs = dt_("s_aff", [SEQ, NEXP], F32, IN)
    affTs = dt_("s_affT", [NEXP, SEQ], F32, IN)
    ys = [dt_("s_y%d" % e, [CAP + 1, D], BF16, IN) for e in range(NEXP)]
    invs = dt_("s_inv", [4, SEQ], I32, IN)
    invTs = dt_("s_invT", [NEXP, 128, 128], I32, IN)
    lsts = [dt_("s_lst%d" % p, [CAP + 128, 2], F32, IN) for p in range(4)]
    thr = dt_("s_thr", [128, 1], F32, IN)
    xpad2 = dt_("s_xpad2", [SEQ + 256, D], F32, IN)

    with ExitStack() as outer:
        S = Sched(nc, outer)
        ctx.S = S
        if depth > 1:
            with ExitStack() as st:
                S.stack = st
                S.prefix = "pz_"
                z = S.sb([128, D], F32, "zpad")
                S.op("dve", lambda e: e.memset(z[:], 0.0), writes=[z])
                S.dma("sp", lambda e: e.dma_start(out=xpad2[0:128, :], in_=z[:]), reads=[z])
                S.dma("sp", lambda e: e.dma_start(out=xpad2[SEQ + 128:SEQ + 256, :], in_=z[:]), reads=[z])
                S.barrier()
        for l in range(depth):
            xin = xpad if l == 0 else xpad2
            last = (l == depth - 1)
            for r in range(4):
                io = dict(cst)
                io.update({"xe": xin[r * TOK:r * TOK + TOK + 256, :], "w_in": w_in[l], "g": g_mix[l],
                           "edges": edges3[0 if r == 0 else (2 if r == 3 else 1)], "sinkb": sinkb[l],
                           "w_ap": w_ap[l], "fT": fTs[r], "maT": maTs[r], "sgbT": sgbTs[r]})
                build_A(ctx, io)
            for g in range(4):
                io = dict(cst)
                io.update({"fin_parts": [fTs[q, g].rearrange("c (t n) -> t c n", n=128) for q in range(4)],
                           "YT": YTs[g]})
                build_B(ctx, io)
            for r in range(4):
                io = dict(cst)
                io.update({"YTi": YTs[:, :, r * TOK:(r + 1) * TOK], "maT": maTs[r], "sgbT": sgbTs[r],
                           "x": xin[128 + r * TOK:128 + (r + 1) * TOK, :], "w_fp": w_fp[l], "w_out": w_out[l],
                           "gb": gffn[l], "w_rt": w_rt[l], "x1": x1s[r * TOK:(r + 1) * TOK, :],
                           "h2": h2s[r * TOK:(r + 1) * TOK, :], "aff": affs[r * TOK:(r + 1) * TOK, :],
                           "affT": affTs[:, r * TOK:(r + 1) * TOK]})
                build_C(ctx, io)
            for eg in range(4):
                io = dict(cst)
                io.update({"affT": affTs[4 * eg:4 * eg + 4, :], "h2": [h2s], "wg": wg[l, 4 * eg:4 * eg + 4],
                           "wu": wu[l, 4 * eg:4 * eg + 4], "wd": wd[l, 4 * eg:4 * eg + 4],
                           "y": [ys[4 * eg + p] for p in range(4)], "lst": lsts, "inv": invs,
                           "invT": invTs[4 * eg:4 * eg + 4], "thr": thr})
                build_E(4, 1, ctx, io)
            for r in range(4):
                io = dict(cst)
                io.update({"x1": x1s[r * TOK:(r + 1) * TOK, :], "invT": invTs[:, :, r * 32:(r + 1) * 32],
                           "ys": ys, "gb": gfin,
                           "out": (out[r * TOK:(r + 1) * TOK, :] if last
                                   else xpad2[128 + r * TOK:128 + (r + 1) * TOK, :])})
                build_F(last, ctx, io)
        S.finish_all("sp")
        print("build_fused: %d instructions" % S.ninst, S.cnt)
    return nc


def host_inputs_fused(inp, depth=DEPTH):
    bucket, rel = _bucket_table()
    i = np.arange(128)
    edges3 = np.zeros((3, 128, 2, 128), np.float32)
    edges3[0, :, 0, :] = NEG
    edges3[2, :, 1, :] = NEG
    common = dict(host_consts_B())
    common.update({
        "biasg": np.ascontiguousarray(inp["rel_bias"][bucket].transpose(0, 3, 1, 2)).astype(np.float32),
        "maskw": np.where(np.abs(rel) <= 128, 0.0, NEG).astype(np.float32),
        "ident": np.eye(128, dtype=np.float32),
        "identf": np.eye(128, dtype=np.float32),
        "ustrict": (i[:, None] < i[None, :]).astype(np.float32),
        "bones": ((i[:, None] // 32) == (i[None, :] // 32)).astype(np.float32),
        "tokid": (i[:, None] * 128 + i[None, :]).astype(np.float32),
        "pidx": i[:, None].astype(np.float32),
        "edges3": edges3,
        "w_in": np.ascontiguousarray(inp["w_in"][:depth]),
        "g_mix": np.ascontiguousarray(inp["g_mix"][:depth].reshape(depth, 8, 128).transpose(0, 2, 1)),
        "sinkb": np.ascontiguousarray(np.broadcast_to(inp["attn_sink"][:depth, None, :], (depth, 128, 8))),
        "w_ap": np.ascontiguousarray(inp["w_attn_proj"][:depth]),
        "w_fp": np.ascontiguousarray(inp["w_fourier_proj"][:depth]),
        "w_out": np.ascontiguousarray(inp["w_out"][:depth]),
        "gffn": np.ascontiguousarray(np.broadcast_to(inp["g_ffn"][:depth, None, :], (depth, 128, D))),
        "w_rt": np.ascontiguousarray(inp["w_router"][:depth]),
        "wg": np.ascontiguousarray(inp["w_exp_gate"][:depth]),
        "wu": np.ascontiguousarray(inp["w_exp_up"][:depth]),
        "wd": np.ascontiguousarray(inp["w_exp_down"][:depth]),
        "gfin": np.ascontiguousarray(np.broadcast_to(inp["g_final"][None, :], (128, D))),
    })
    xp = []
    for b in range(NB):
        t = np.zeros((SEQ + 256, D), np.float32)
        t[128:128 + SEQ] = inp["x"][b]
        xp.append(t)
    maps = []
    for c in range(NCORES):
        m = dict(common)
        m["xpad"] = xp[c // 4]
        maps.append(m)
    return maps


def kernel_fused(depth=DEPTH, **inputs):
    inp = {k: np.asarray(v) for k, v in inputs.items()}
    res = _run(build_fused(depth), host_inputs_fused(inp, depth))
    return np.stack([np.asarray(res[0]["out"]), np.asarray(res[4]["out"])]).astype(np.float32)


def kernel(**inputs):
    return kernel_fused(DEPTH, **inputs)
```
